# Optimizing a Trainium2 kernel written in Bass

```python
import jax, jax.numpy as jnp
from jax import lax
import numpy as np

D_MODEL = 1024
BATCH = 32
SEQ = 2048
DEPTH = 4

ATT_PATTERNS = ((128, 1), (512, 4), (2048, 16))
N_ATT_GROUPS = 3
ATT_HEADS_PER_GROUP = 4
ATT_HEAD_DIM = 128
ATT_QKV = N_ATT_GROUPS * ATT_HEADS_PER_GROUP * ATT_HEAD_DIM
ATT_OUT = ATT_HEADS_PER_GROUP * ATT_HEAD_DIM
ATT_BLOCK = 128
RET_HEADS = 4
RET_QK_DIM = D_MODEL // RET_HEADS
RET_V_DIM = D_MODEL // RET_HEADS
RET_QK = RET_HEADS * RET_QK_DIM
RET_WIDTH = RET_HEADS * RET_V_DIM
RET_CHUNK = 128
ROPE_BASE = 10000.0
EPS = 1e-6
NEG_INF = -1e30
SPLIT_SIZES = (ATT_QKV, ATT_QKV, ATT_QKV, ATT_OUT, RET_QK, RET_QK, RET_WIDTH, RET_WIDTH, D_MODEL, D_MODEL)
IN_WIDTH = sum(SPLIT_SIZES)
SPLIT_POINTS = tuple(int(v) for v in np.cumsum(SPLIT_SIZES)[:-1])

kernel_name = "hybrid_dilated_attn_retention_adaln"


def rmsnorm(x, w):
    xf = x.astype(jnp.float32)
    y = xf * lax.rsqrt(jnp.mean(xf * xf, axis=-1, keepdims=True) + EPS)
    return (y * w.astype(jnp.float32)).astype(x.dtype)


def dilated_window_attention(q, k, v, window, dil):
    B, S, G, Dh = q.shape
    L = S // dil
    span = window // dil
    blk = min(ATT_BLOCK, L)
    nb = -(-L // blk)
    Lp = nb * blk

    def to_sub(t):
        t = t.astype(jnp.float32).reshape(B, L, dil, G, Dh).transpose(0, 2, 3, 1, 4)
        t = jnp.pad(t, ((0, 0), (0, 0), (0, 0), (0, Lp - L), (0, 0)))
        return t.reshape(B, dil, G, nb, blk, Dh)

    def with_prev(t):
        prev = jnp.pad(t, ((0, 0), (0, 0), (0, 0), (1, 0), (0, 0), (0, 0)))[:, :, :, :-1]
        return jnp.concatenate([prev, t], axis=-2)

    qs = to_sub(q)
    kk = with_prev(to_sub(k))
    vv = with_prev(to_sub(v))
    s = jnp.einsum('bcgnqd,bcgnkd->bcgnqk', qs, kk) * (Dh ** -0.5)
    qi = jnp.arange(nb)[:, None, None] * blk + jnp.arange(blk)[None, :, None]
    ki = jnp.arange(nb)[:, None, None] * blk - blk + jnp.arange(2 * blk)[None, None, :]
    dist = qi - ki
    valid = (ki >= 0) & (dist >= 0) & (dist <= span)
    s = jnp.where(valid, s, NEG_INF)
    m = jnp.max(s, axis=-1, keepdims=True)
    p = jnp.exp(s - m)
    l = jnp.sum(p, axis=-1, keepdims=True)
    o = jnp.einsum('bcgnqk,bcgnkd->bcgnqd', p, vv) / l
    lse = (m + jnp.log(l))[..., 0]
    o = o.reshape(B, dil, G, Lp, Dh)[:, :, :, :L].transpose(0, 3, 1, 2, 4).reshape(B, S, G, Dh)
    lse = lse.reshape(B, dil, G, Lp)[..., :L].transpose(0, 3, 1, 2).reshape(B, S, G)
    return o, lse


def rotary(t, positions):
    d = t.shape[-1]
    half = d // 2
    theta = ROPE_BASE ** (-jnp.arange(half, dtype=jnp.float32) / half)
    ang = positions.astype(jnp.float32)[..., None] * theta
    cos, sin = jnp.cos(ang)[:, :, None, :], jnp.sin(ang)[:, :, None, :]
    tf = t.astype(jnp.float32)
    t1, t2 = tf[..., :half], tf[..., half:]
    return jnp.concatenate([t1 * cos - t2 * sin, t2 * cos + t1 * sin], axis=-1)


def retention(q, k, v, positions, gn_w):
    B, S, H, dk = q.shape
    dv = v.shape[-1]
    q = rotary(q, positions)
    k = rotary(k, positions) * (dk ** -0.5)
    C = RET_CHUNK
    N = S // C

    def chunks(t):
        return t.astype(jnp.float32).reshape(B, N, C, H, t.shape[-1]).transpose(1, 0, 3, 2, 4)

    log_g = jnp.log1p(-jnp.exp2(-5.0 - jnp.arange(H, dtype=jnp.float32)))
    idx = jnp.arange(C, dtype=jnp.float32)
    diff = idx[:, None] - idx[None, :]
    inner_decay = jnp.where(diff >= 0, jnp.exp(log_g[:, None, None] * jnp.maximum(diff, 0.0)), 0.0)
    q_decay = jnp.exp(log_g[:, None] * (idx + 1.0))
    k_decay = jnp.exp(log_g[:, None] * (C - 1.0 - idx))
    chunk_decay = jnp.exp(log_g * C)

    def step(state, qkv):
        qc, kc, vc = qkv
        att = jnp.einsum('bhnd,bhmd->bhnm', qc, kc) * inner_decay
        out = (jnp.einsum('bhnm,bhme->bhne', att, vc)
               + jnp.einsum('bhnd,bhde->bhne', qc, state) * q_decay[:, :, None])
        state = (state * chunk_decay[:, None, None]
                 + jnp.einsum('bhmd,bhme->bhde', kc * k_decay[:, :, None], vc))
        return state, out

    state0 = jnp.zeros((B, H, dk, dv), jnp.float32)
    _, out = lax.scan(step, state0, (chunks(q), chunks(k), chunks(v)))
    out = out.transpose(1, 0, 3, 2, 4).reshape(B, S, H, dv)
    mu = jnp.mean(out, axis=-1, keepdims=True)
    var = jnp.mean(jnp.square(out - mu), axis=-1, keepdims=True)
    out = (out - mu) * lax.rsqrt(var + EPS)
    return out.reshape(B, S, H * dv) * gn_w.astype(jnp.float32)


def hybrid_layer(x, c_act, positions, norm_w, w_ada, b_ada, w_in, ret_gn_w, w_proj_attn, w_proj_ret, w_out):
    B, S, D = x.shape
    mod = c_act @ w_ada + b_ada
    shift, scale, gate = jnp.split(mod, 3, axis=-1)
    h = rmsnorm(x, norm_w) * (1.0 + scale[:, None, :]) + shift[:, None, :]
    proj = h @ w_in
    qa, ka, va, za, qr, kr, vr, zr, ga, gr = jnp.split(proj, SPLIT_POINTS, axis=-1)

    att_shape = (B, S, N_ATT_GROUPS, ATT_HEADS_PER_GROUP, ATT_HEAD_DIM)
    qa, ka, va = qa.reshape(att_shape), ka.reshape(att_shape), va.reshape(att_shape)
    outs, lses = [], []
    for g, (window, dil) in enumerate(ATT_PATTERNS):
        o, lse = dilated_window_attention(qa[:, :, g], ka[:, :, g], va[:, :, g], window, dil)
        outs.append(o)
        lses.append(lse)
    wts = jax.nn.softmax(jnp.stack(lses, axis=0), axis=0)
    o_att = jnp.sum(wts[..., None] * jnp.stack(outs, axis=0), axis=0)
    y_att = o_att.reshape(B, S, ATT_OUT).astype(x.dtype) * jax.nn.silu(za)

    ret_qk = (B, S, RET_HEADS, RET_QK_DIM)
    o_ret = retention(qr.reshape(ret_qk), kr.reshape(ret_qk), vr.reshape(B, S, RET_HEADS, RET_V_DIM),
                      positions, ret_gn_w)
    y_ret = o_ret.astype(x.dtype) * jax.nn.silu(zr)

    merged = jax.nn.sigmoid(ga) * (y_att @ w_proj_attn) + jax.nn.sigmoid(gr) * (y_ret @ w_proj_ret)
    return x + gate[:, None, :] * (merged @ w_out)


def setup_inputs(seed: int = 0) -> dict:
    key = jax.random.key(seed)
    ks = jax.random.split(key, 12)
    f32 = jnp.float32
    D = D_MODEL
    x = jax.random.normal(ks[0], (BATCH, SEQ, D), f32)
    c = jax.random.normal(ks[1], (BATCH, D), f32)
    positions = jnp.tile(jnp.arange(SEQ, dtype=jnp.int32)[None, :], (BATCH, 1))
    norm_w = 1.0 + 0.01 * jax.random.normal(ks[2], (DEPTH, D), f32)
    w_ada = 0.5 * jax.random.normal(ks[3], (DEPTH, D, 3 * D), f32) * D ** -0.5
    b_ada = 0.01 * jax.random.normal(ks[4], (DEPTH, 3 * D), f32)
    w_in = jax.random.normal(ks[5], (DEPTH, D, IN_WIDTH), f32) * D ** -0.5
    ret_gn_w = 1.0 + 0.01 * jax.random.normal(ks[6], (DEPTH, RET_WIDTH), f32)
    w_proj_attn = jax.random.normal(ks[7], (DEPTH, ATT_OUT, D), f32) * ATT_OUT ** -0.5
    w_proj_ret = jax.random.normal(ks[8], (DEPTH, RET_WIDTH, D), f32) * RET_WIDTH ** -0.5
    w_out = jax.random.normal(ks[9], (DEPTH, D, D), f32) * D ** -0.5
    final_norm_w = 1.0 + 0.01 * jax.random.normal(ks[10], (D,), f32)
    return {"x": x, "c": c, "positions": positions, "norm_w": norm_w, "w_ada": w_ada, "b_ada": b_ada,
            "w_in": w_in, "ret_gn_w": ret_gn_w, "w_proj_attn": w_proj_attn, "w_proj_ret": w_proj_ret,
            "w_out": w_out, "final_norm_w": final_norm_w}


def reference(x, c, positions, norm_w, w_ada, b_ada, w_in, ret_gn_w, w_proj_attn, w_proj_ret, w_out, final_norm_w):
    c_act = jax.nn.silu(c)
    for l in range(DEPTH):
        x = hybrid_layer(x, c_act, positions, norm_w[l], w_ada[l], b_ada[l], w_in[l], ret_gn_w[l],
                         w_proj_attn[l], w_proj_ret[l], w_out[l])
    return rmsnorm(x, final_norm_w)
```

```python
import math
import numpy as np
from contextlib import ExitStack
import concourse.bass as bass
import concourse.mybir as mybir
from concourse.bass_utils import run_bass_kernel_spmd

F32 = mybir.dt.float32
BF16 = mybir.dt.bfloat16
I32 = mybir.dt.int32
AF = mybir.ActivationFunctionType
ALU = mybir.AluOpType

S = 2048
D = 1024
INW = 11264
EPS = 1e-6
NCORES = 8
NS = 10
PI = math.pi


class Buf:
    __slots__ = ("w", "r")

    def __init__(self):
        self.w = {}
        self.r = {}


class Eng:
    def __init__(self, name, sem):
        self.name = name
        self.sem = sem
        self.count = 0
        self.waited = {}
        self.items = []


class Chan:
    def __init__(self, sem):
        self.sem = sem
        self.count = 0


class Prog:
    def __init__(self, nc, sems):
        self.nc = nc
        self.free_sems = list(sems)
        self.E = {n: Eng(n, self.free_sems.pop()) for n in ("pe", "act", "dve", "pool", "sp")}
        self.chans = []

    def chan(self):
        c = Chan(self.free_sems.pop())
        self.chans.append(c)
        return c

    def _deps(self, reads, writes):
        deps = {}
        for b in reads:
            for s, v in b.w.items():
                if deps.get(s, 0) < v:
                    deps[s] = v
        for b in writes:
            for s, v in b.w.items():
                if deps.get(s, 0) < v:
                    deps[s] = v
            for s, v in b.r.items():
                if deps.get(s, 0) < v:
                    deps[s] = v
        return deps

    def _emit_waits(self, e, deps, skip_own=False):
        for s, v in deps.items():
            if skip_own and s is e.sem:
                continue
            if e.waited.get(s, 0) < v:
                e.items.append(("w", s, v))
                e.waited[s] = v

    def _commit(self, tok, reads, writes):
        s, v = tok
        for b in reads:
            if b.r.get(s, 0) < v:
                b.r[s] = v
        for b in writes:
            b.w = {s: v}
            b.r = {}

    def op(self, eng, insts, reads=(), writes=()):
        if isinstance(insts, tuple):
            insts = [insts]
        e = self.E[eng]
        self._emit_waits(e, self._deps(reads, writes), skip_own=(eng == "pe"))
        e.count += 1
        e.items.append(("o", insts, e.sem, 1))
        self._commit((e.sem, e.count), reads, writes)

    def dma(self, eng, out, in_, chan, reads=(), writes=(), nonc=False):
        e = self.E[eng]
        self._emit_waits(e, self._deps(reads, writes))
        chan.count += 16
        e.items.append(("d" if nonc else "o", [("dma_start", dict(out=out, in_=in_))], chan.sem, 16))
        self._commit((chan.sem, chan.count), reads, writes)

    def barrier(self, engs=("pe", "act", "dve", "sp")):
        deps = {}
        for n in engs:
            e = self.E[n]
            if e.count and n != "sp":
                deps[e.sem] = e.count
        for n in engs:
            self._emit_waits(self.E[n], deps)

    def final_wait(self, eng, chans):
        e = self.E[eng]
        self._emit_waits(e, {c.sem: c.count for c in chans if c.count})

    def replay(self, block):
        nc = self.nc

        def run(e, engobj):
            for it in e.items:
                if it[0] == "w":
                    engobj.wait_ge(it[1], it[2])
                elif it[0] == "d":
                    with nc.allow_non_contiguous_dma(reason="tiny parameter layout loads"):
                        for name, kw in it[1]:
                            inst = getattr(engobj, name)(**kw)
                    inst.then_inc(it[2], it[3])
                else:
                    for name, kw in it[1]:
                        inst = getattr(engobj, name)(**kw)
                    inst.then_inc(it[2], it[3])

        @block.tensor
        def _(eng):
            run(self.E["pe"], eng)

        @block.scalar
        def _(eng):
            run(self.E["act"], eng)

        @block.vector
        def _(eng):
            run(self.E["dve"], eng)

        @block.gpsimd
        def _(eng):
            run(self.E["pool"], eng)

        @block.sync
        def _(eng):
            run(self.E["sp"], eng)


def host_consts():
    ident = np.eye(128, dtype=np.float32)
    ones = np.ones((128, 128), dtype=np.float32)
    j = np.arange(128)[:, None]
    i = np.arange(128)[None, :]
    mask = np.zeros((128, 256), dtype=np.float32)
    mask[:, 0:128] = np.where(i >= j, 0.0, -30000.0)
    mask[:, 128:256] = np.where(i <= j, 0.0, -30000.0)
    gam = 1.0 - np.exp2(-5.0 - np.arange(4, dtype=np.float64))
    lg = np.log(gam)
    m = np.arange(128)[:, None]
    n = np.arange(128)[None, :]
    amask = np.zeros((128, 4, 128), dtype=np.float32)
    qdec = np.zeros((128, 4), dtype=np.float32)
    kdec = np.zeros((128, 4), dtype=np.float32)
    for h in range(4):
        amask[:, h, :] = np.where(n >= m, np.exp(lg[h] * np.maximum(n - m, 0)), 0.0) / 16.0
        qdec[:, h] = np.exp(lg[h] * (np.arange(128) + 1.0))
        kdec[:, h] = np.exp(lg[h] * (127.0 - np.arange(128))) / 16.0
    cdec = [float(np.exp(lg[h] * 128.0)) for h in range(4)]
    theta = (np.float32(10000.0) ** (-(np.arange(128, dtype=np.float32) / np.float32(128.0)))).astype(np.float32)[:, None]
    cf = np.concatenate([ident, ones, amask.reshape(128, 512), qdec, kdec, theta,
                         np.full((128, 1), EPS, np.float32)], axis=1)
    cb = np.concatenate([ident, ones, mask], axis=1)
    return np.ascontiguousarray(cf), np.ascontiguousarray(cb), cdec


CF_W = 128 + 128 + 512 + 4 + 4 + 1 + 1
_, _, CDEC = host_consts()


def MM(out, lhsT, rhs, start=True, stop=True, sg=False):
    kw = dict(out=out, lhsT=lhsT, rhs=rhs, start=start, stop=stop)
    if sg:
        kw["skip_group_check"] = True
    return ("matmul", kw)


def TR(out, in_, ident):
    return ("transpose", dict(out=out, in_=in_, identity=ident))


def ACT(out, in_, func, scale=None, bias=None):
    kw = dict(out=out, in_=in_, func=func)
    if scale is not None:
        kw["scale"] = scale
    if bias is not None:
        kw["bias"] = bias
    return ("activation", kw)


def TT(out, in0, in1, op):
    return ("tensor_tensor", dict(out=out, in0=in0, in1=in1, op=op))


def TS(out, in0, s1, s2, op0, op1=None):
    kw = dict(out=out, in0=in0, scalar1=s1, scalar2=s2, op0=op0)
    if op1 is not None:
        kw["op1"] = op1
    return ("tensor_scalar", kw)


def STT(out, in0, scalar, in1, op0, op1):
    return ("scalar_tensor_tensor", dict(out=out, in0=in0, scalar=scalar, in1=in1, op0=op0, op1=op1))


def CP(out, in_):
    return ("tensor_copy", dict(out=out, in_=in_))


def RCP(out, in_):
    return ("reciprocal", dict(out=out, in_=in_))


def build(depth, nseq, want_xo):
    nc = bass.Bass("TRN2", target_bir_lowering=False)
    dt_ = nc.dram_tensor
    x_d = dt_("x", [nseq, S, D], F32, kind="ExternalInput").ap()
    c_d = dt_("c", [nseq, D], F32, kind="ExternalInput").ap()
    pos_d = dt_("pos", [nseq, S], I32, kind="ExternalInput").ap()
    nw_d = dt_("norm_w", [depth, D], F32, kind="ExternalInput").ap()
    wada_d = dt_("w_ada", [depth, D, 3 * D], F32, kind="ExternalInput").ap()
    bada_d = dt_("b_ada", [depth, 3 * D], F32, kind="ExternalInput").ap()
    win_d = dt_("w_in", [depth, D, INW], F32, kind="ExternalInput").ap()
    gnw_d = dt_("gn_w", [depth, D], F32, kind="ExternalInput").ap()
    wpa_d = dt_("w_pa", [depth, 512, D], F32, kind="ExternalInput").ap()
    wpr_d = dt_("w_pr", [depth, D, D], F32, kind="ExternalInput").ap()
    wout_d = dt_("w_out", [depth, D, D], F32, kind="ExternalInput").ap()
    fnw_d = dt_("fnw", [D], F32, kind="ExternalInput").ap()
    cf_d = dt_("cf", [128, CF_W], F32, kind="ExternalInput").ap()
    cb_d = dt_("cb", [128, 512], F32, kind="ExternalInput").ap()
    y_d = dt_("y", [nseq, S, D], F32, kind="ExternalOutput").ap()
    xo_d = dt_("xo", [nseq, S, D], F32, kind="ExternalOutput").ap() if want_xo else None

    BN_S = nc.vector.BN_STATS_DIM
    BN_A = nc.vector.BN_AGGR_DIM
    es = ExitStack()
    with es:
        sems = [es.enter_context(nc.semaphore(f"s{i}")) for i in range(64)]
        sb = lambda name, shape, dt: es.enter_context(nc.sbuf_tensor("sb_" + name, shape, dt))
        xT = sb("xT", [128, 8, S], F32)
        hT = sb("hT", [128, 8, S], BF16)
        yatt = sb("yatt", [128, 4, S], BF16)
        yret = sb("yret", [128, 8, S], BF16)
        wring = sb("wring", [128, NS, 8, 128], BF16)
        cf = sb("cf", [128, CF_W], F32)
        cb = sb("cb", [128, 512], BF16)
        nwT = sb("nwT", [128, depth, 8], F32)
        gwT = sb("gwT", [128, depth, 8], F32)
        baT = sb("baT", [128, depth, 24], F32)
        fnT = sb("fnT", [128, 8], F32)
        cA = sb("cA", [128, 8, nseq], F32)
        modT = sb("modT", [128, depth, 24, nseq], F32)
        aT = sb("aT", [128, depth, 8, nseq], F32)
        AR = 9984
        arena = sb("arena", [128, AR], F32)
        psf = [es.enter_context(nc.psum_tensor(f"ps{i}", [128, 512], F32)) for i in range(7)]
        psb = es.enter_context(nc.psum_tensor("psb", [128, 1024], BF16))

        identf = cf[:, 0:128]
        onesf = cf[:, 128:256]
        amask = cf[:, 256:768].rearrange("p (h n) -> p h n", h=4)
        qdec = cf[:, 768:772]
        kdec = cf[:, 772:776]
        theta = cf[:, 776:777]
        epsc = cf[:, 777:778]
        identb = cb[:, 0:128]
        onesb = cb[:, 128:256]
        maskb = cb[:, 256:512]

        P = Prog(nc, sems)
        B = Buf
        b_xT, b_hT, b_yatt, b_yret = B(), B(), B(), B()
        b_const, b_cb, b_small = B(), B(), B()
        b_ps = [B() for _ in range(7)]
        b_psb = B()
        b_xst = [B(), B()]
        ch_xst = [P.chan(), P.chan()]
        ch_cf, ch_cb, ch_posi = P.chan(), P.chan(), P.chan()
        ch_out = [P.chan(), P.chan()]
        b_ring = [B() for _ in range(NS)]
        ch_ring = [P.chan() for _ in range(NS)]
        ring_pos = [0]

        def carve(off, n, dt=F32):
            if dt == F32:
                assert off + n <= AR
                return arena[:, off:off + n]
            assert off + n // 2 <= AR
            return arena[:, off:off + n // 2].bitcast(BF16)

        def wload(src_ap, kc):
            slot = ring_pos[0] % NS
            ring_pos[0] += 1
            P.dma("pool", wring[:, slot, 0:kc, :], src_ap.rearrange("(c p) n -> p c n", p=128),
                  ch_ring[slot], writes=[b_ring[slot]])
            return slot

        def ring_align2():
            if ring_pos[0] % 2:
                ring_pos[0] += 1

        P.dma("sp", cf[:], cf_d[:, :], ch_cf, writes=[b_const])
        P.dma("pool", cb[:], cb_d[:, :], ch_cb, writes=[b_cb])
        b_p1, b_p2, b_p3, b_p4, b_p5 = B(), B(), B(), B(), B()
        ch_p = [P.chan() for _ in range(5)]
        P.dma("sp", nwT[:], nw_d.rearrange("l (c p) -> p l c", p=128), ch_p[0], writes=[b_p1], nonc=True)
        P.dma("sp", baT[:], bada_d.rearrange("l (c p) -> p l c", p=128), ch_p[1], writes=[b_p2], nonc=True)
        P.dma("sp", fnT[:], fnw_d.rearrange("(c p) -> p c", p=128), ch_p[2], writes=[b_p3], nonc=True)
        P.dma("sp", cA[:], c_d.rearrange("b (c p) -> p c b", p=128), ch_p[3], writes=[b_p4], nonc=True)
        P.dma("sp", gwT[:], gnw_d.rearrange("l (c p) -> p l c", p=128), ch_p[4], writes=[b_p5], nonc=True)
        P.op("act", ACT(cA[:], cA[:], AF.Silu), reads=[b_p4], writes=[b_p4])

        wst = [carve(i * 4096, 4096).rearrange("p (c n) -> p c n", c=8) for i in range(2)]
        b_wst = [B(), B()]
        ch_wst = [P.chan(), P.chan()]
        b_mod = B()
        it = 0
        for l in range(depth):
            for jg in range(6):
                k = it % 2
                it += 1
                P.dma("sp", wst[k], wada_d[l, :, jg * 512:(jg + 1) * 512].rearrange("(c p) n -> p c n", p=128),
                      ch_wst[k], writes=[b_wst[k]])
                mm = []
                for j4 in range(4):
                    for kc in range(8):
                        mm.append(MM(psf[0][:, j4 * nseq:(j4 + 1) * nseq], wst[k][:, kc, j4 * 128:(j4 + 1) * 128],
                                     cA[:, kc, :], start=(kc == 0), stop=(kc == 7)))
                P.op("pe", mm, reads=[b_wst[k], b_p4], writes=[b_ps[0]])
                P.op("dve", TT(modT[:, l, jg * 4:(jg + 1) * 4, :],
                               psf[0][:, 0:4 * nseq].rearrange("p (j b) -> p j b", j=4),
                               baT[:, l, jg * 4:(jg + 1) * 4].unsqueeze(2).broadcast_to([128, 4, nseq]), ALU.add),
                     reads=[b_ps[0], b_p2], writes=[b_mod])
        for l in range(depth):
            P.op("dve", STT(aT[:, l, :, :], modT[:, l, 8:16, :], 1.0,
                            nwT[:, l, :].unsqueeze(2).broadcast_to([128, 8, nseq]), ALU.add, ALU.mult),
                 reads=[b_mod, b_p1], writes=[b_small])

        def stats_rstd(t, rt, b_rt, sq, b_sq):
            ts = slice(t * 512, (t + 1) * 512)
            for c in range(8):
                k = c % 2
                P.op("act", ACT(sq[k], xT[:, c, ts], AF.Square), reads=[b_xT], writes=[b_sq[k]])
                P.op("pe", MM(psf[6][:, :], onesf, sq[k], start=(c == 0), stop=(c == 7)),
                     reads=[b_sq[k], b_const], writes=[b_ps[6]])
            P.op("act", ACT(rt, psf[6][:, :], AF.Sqrt, scale=1.0 / D, bias=epsc), reads=[b_ps[6], b_const], writes=[b_rt])
            P.op("dve", RCP(rt, rt), reads=[b_rt], writes=[b_rt])

        def proj_feat(slot, t, bank, kc=8, src=None, bs=None):
            src = hT if src is None else src
            bs = b_hT if bs is None else bs
            mm = [MM(psf[bank][:, :], wring[:, slot, c, :], src[:, c, t * 512:(t + 1) * 512],
                     start=(c == 0), stop=(c == kc - 1)) for c in range(kc)]
            P.op("pe", mm, reads=[b_ring[slot], bs], writes=[b_ps[bank]])

        evac_flip = [0]

        def evac_copy(out, in_, reads, writes, scale=None):
            evac_flip[0] ^= 1
            if evac_flip[0]:
                P.op("act", ACT(out, in_, AF.Copy, scale=scale), reads=reads, writes=writes)
            elif scale is None:
                P.op("dve", CP(out, in_), reads=reads, writes=writes)
            else:
                P.op("dve", TS(out, in_, scale, None, ALU.mult), reads=reads, writes=writes)

        for b in range(nseq):
            P.barrier()
            xst = [carve(0, 1024), carve(1024, 1024)]
            for tt in range(16):
                k = tt % 2
                P.dma("sp", xst[k], x_d[b, tt * 128:(tt + 1) * 128, :], ch_xst[k], writes=[b_xst[k]])
                for half in range(2):
                    bank = half
                    tr = [TR(psf[bank][:, q * 128:(q + 1) * 128], xst[k][:, (half * 4 + q) * 128:(half * 4 + q + 1) * 128], identf)
                          for q in range(4)]
                    P.op("pe", tr, reads=[b_xst[k], b_const], writes=[b_ps[bank]])
                    evac_copy(xT[:, half * 4:half * 4 + 4, tt * 128:(tt + 1) * 128],
                              psf[bank][:, :].rearrange("p (q n) -> p q n", q=4), [b_ps[bank]], [b_xT])

            for l in range(depth):
                P.barrier()
                sq = [carve(0, 512), carve(512, 512)]
                b_sq = [B(), B()]
                rt = carve(1024, 512)
                b_rt = B()
                tmp = [carve(1536, 512), carve(2048, 512)]
                b_tmp = [B(), B()]
                for t in range(4):
                    ts = slice(t * 512, (t + 1) * 512)
                    stats_rstd(t, rt, b_rt, sq, b_sq)
                    for c in range(8):
                        k = c % 2
                        P.op("dve", TT(tmp[k], xT[:, c, ts], rt, ALU.mult), reads=[b_xT, b_rt], writes=[b_tmp[k]])
                        P.op("act", ACT(hT[:, c, ts], tmp[k], AF.Identity, scale=aT[:, l, c, b:b + 1], bias=modT[:, l, c, b:b + 1]),
                             reads=[b_tmp[k], b_small, b_mod], writes=[b_hT])

                P.barrier()
                sza = carve(0, S, BF16)
                Uacc = carve(1024, S)
                Lacc = carve(3072, S)
                pT = [carve(5120, 512, BF16), carve(5376, 512, BF16)]
                rl = carve(5632, 512)
                qkv = []
                for i in range(2):
                    qkv.append(dict(q=yret[:, 4 * i, :], k=yret[:, 4 * i + 1, :], vT=yret[:, 4 * i + 2, :],
                                    v=yret[:, 4 * i + 3, :].rearrange("p (k n) -> p k n", k=16),
                                    bq=B(), bk=B(), bvT=B(), bv=B()))
                b_pT = [B(), B()]
                b_sza, b_U, b_L, b_rl = B(), B(), B(), B()
                gi = 0
                sc_i = 0
                for s in range(4):
                    slot = wload(win_d[l, :, 4608 + s * 128: 4608 + (s + 1) * 128], 8)
                    for t in range(4):
                        bank = t % 2
                        proj_feat(slot, t, bank)
                        P.op("act", ACT(sza[:, t * 512:(t + 1) * 512], psf[bank][:, :], AF.Silu),
                             reads=[b_ps[bank]], writes=[b_sza])
                    for g, dil in enumerate((1, 4, 16)):
                        Q = qkv[gi % 2]
                        gi += 1
                        L = S // dil
                        CL = L // 128
                        for name, off, bb, scale in (("q", 0, Q["bq"], 128 ** -0.5), ("k", 1536, Q["bk"], None),
                                                     ("vT", 3072, Q["bvT"], None)):
                            c0 = off + g * 512 + s * 128
                            slot = wload(win_d[l, :, c0:c0 + 128], 8)
                            dst = Q[name]
                            for t in range(4):
                                bank = t % 2
                                proj_feat(slot, t, bank)
                                m0 = t * 512 // dil
                                mw = 512 // dil
                                if dil == 1:
                                    o_ap = dst[:, t * 512:(t + 1) * 512]
                                    i_ap = psf[bank][:, :]
                                else:
                                    o_ap = dst.rearrange("p (r m) -> p r m", r=dil)[:, :, m0:m0 + mw]
                                    i_ap = psf[bank][:, :].rearrange("p (m r) -> p r m", r=dil)
                                evac_copy(o_ap, i_ap, [b_ps[bank]], [bb], scale=scale)
                        for q4 in range(4):
                            tr = [TR(psb[:, q * 128:(q + 1) * 128], Q["vT"][:, (q4 * 4 + q) * 128:(q4 * 4 + q + 1) * 128], identb)
                                  for q in range(4)]
                            P.op("pe", tr, reads=[Q["bvT"], b_cb], writes=[b_psb])
                            evac_copy(Q["v"][:, q4 * 4:q4 * 4 + 4, :], psb[:, 0:512].rearrange("p (q n) -> p q n", q=4),
                                      [b_psb], [Q["bv"]])
                        for Bk in range(4):
                            q_lo = 4 * Bk
                            kb_lo = q_lo - 1 if (q_lo % CL) != 0 else q_lo
                            first = True
                            for kb in range(kb_lo, q_lo + 4):
                                qbs = []
                                if kb >= q_lo:
                                    qbs.append(kb)
                                if (kb + 1) % CL != 0 and kb + 1 <= q_lo + 3:
                                    qbs.append(kb + 1)
                                n = 128 * len(qbs)
                                q0 = qbs[0]
                                m_lo = 0 if qbs[0] == kb else 128
                                sb_i = sc_i % 2
                                sc_i += 1
                                bankS = 2 + sb_i
                                P.op("pe", [MM(psf[bankS][:, 0:n], Q["k"][:, kb * 128:(kb + 1) * 128],
                                               Q["q"][:, q0 * 128:q0 * 128 + n], start=True, stop=False),
                                            MM(psf[bankS][:, 0:n], identb, maskb[:, m_lo:m_lo + n], start=False, stop=True)],
                                     reads=[Q["bq"], Q["bk"], b_cb], writes=[b_ps[bankS]])
                                P.op("act", ACT(pT[sb_i][:, 0:n], psf[bankS][:, 0:n], AF.Exp),
                                     reads=[b_ps[bankS]], writes=[b_pT[sb_i]])
                                last = (kb == q_lo + 3)
                                col = (q0 - q_lo) * 128
                                P.op("pe", [MM(psf[4][:, col:col + n], Q["v"][:, kb, :], pT[sb_i][:, 0:n], start=first, stop=last, sg=True),
                                            MM(psf[5][:, col:col + n], onesb, pT[sb_i][:, 0:n], start=first, stop=last, sg=True)],
                                     reads=[Q["bv"], b_pT[sb_i], b_cb], writes=[b_ps[4], b_ps[5]])
                                first = False
                            for acc, bacc, bank in ((Uacc, b_U, 4), (Lacc, b_L, 5)):
                                if dil == 1:
                                    o_ap = acc[:, Bk * 512:(Bk + 1) * 512]
                                    i_ap = psf[bank][:, :]
                                elif dil == 4:
                                    o_ap = acc.rearrange("p (m r) -> p r m", r=4)[:, Bk, :]
                                    i_ap = psf[bank][:, :]
                                else:
                                    o_ap = acc.rearrange("p (m r) -> p r m", r=16)[:, 4 * Bk:4 * Bk + 4, :]
                                    i_ap = psf[bank][:, :].rearrange("p (r m) -> p r m", r=4)
                                if g == 0:
                                    P.op("dve", CP(o_ap, i_ap), reads=[b_ps[bank]], writes=[bacc])
                                else:
                                    P.op("dve", TT(o_ap, i_ap, o_ap, ALU.add), reads=[b_ps[bank], bacc], writes=[bacc])
                    for t in range(4):
                        ts = slice(t * 512, (t + 1) * 512)
                        P.op("dve", RCP(rl, Lacc[:, ts]), reads=[b_L], writes=[b_rl])
                        P.op("dve", TT(rl, Uacc[:, ts], rl, ALU.mult), reads=[b_U, b_rl], writes=[b_rl])
                        P.op("dve", TT(yatt[:, s, ts], rl, sza[:, ts], ALU.mult), reads=[b_rl, b_sza], writes=[b_yatt])

                P.barrier()
                HS = 512
                NCH = HS // 128
                cosT = carve(0, S)
                sinT = carve(2048, S)
                qT = carve(4096, 2 * HS, BF16).rearrange("p (e n) -> p e n", e=2)
                kT = carve(4608, 2 * HS, BF16).rearrange("p (e n) -> p e n", e=2)
                kd = carve(5120, NCH * 256, BF16).rearrange("p (c n) -> p c n", c=NCH)
                vr = carve(5632, NCH * 256, BF16).rearrange("p (c n) -> p c n", c=NCH)
                szr = carve(6144, NCH * 256, BF16).rearrange("p (c n) -> p c n", c=NCH)
                t1 = [carve(6656, 512), carve(7168, 512)]
                stf = carve(7680, 512)
                stb = [carve(8192, 512, BF16).rearrange("p (e n) -> p e n", e=2),
                       carve(8448, 512, BF16).rearrange("p (e n) -> p e n", e=2)]
                attS = [carve(8704, 128, BF16), carve(8768, 128, BF16)]
                o1 = carve(8832, 256)
                o2 = carve(9088, 256)
                yr = carve(9344, 256, BF16)
                yr2 = carve(9472, 256)
                st6 = carve(9728, 8)
                mv = carve(9736, 4)
                rs = carve(9740, 1)
                nmr = carve(9742, 1)
                b_cs = B()
                b_qT, b_kT, b_kd, b_vr, b_szr = B(), B(), B(), B(), B()
                b_t1 = [B(), B()]
                b_stf = B()
                b_stb = [B(), B()]
                b_attS = [B(), B()]
                b_o1, b_o2, b_yr, b_yr2, b_st = B(), B(), B(), B(), B()
                posi = stf.bitcast(I32)
                ang, ang2 = t1[0], t1[1]
                for t in range(4):
                    ts = slice(t * 512, (t + 1) * 512)
                    P.dma("sp", posi, pos_d[b:b + 1, ts].broadcast_to([128, 512]), ch_posi, writes=[b_stf])
                    P.op("dve", CP(ang, posi), reads=[b_stf], writes=[b_t1[0]])
                    P.op("dve", TS(ang, ang, theta, None, ALU.mult), reads=[b_t1[0], b_const], writes=[b_t1[0]])
                    MAGIC = 12582912.0
                    for dst, use_shift in ((sinT, False), (cosT, True)):
                        srcang = ang
                        bsrc = b_t1[0]
                        if use_shift:
                            P.op("dve", TS(stf, ang, PI / 2, None, ALU.add), reads=[b_t1[0]], writes=[b_stf])
                            srcang = stf
                            bsrc = b_stf
                        P.op("dve", TS(ang2, srcang, 1.0 / (2 * PI), MAGIC, ALU.mult, ALU.add), reads=[bsrc], writes=[b_t1[1]])
                        P.op("dve", TS(ang2, ang2, -MAGIC, None, ALU.add), reads=[b_t1[1]], writes=[b_t1[1]])
                        P.op("dve", STT(ang2, ang2, -2 * PI, srcang, ALU.mult, ALU.add), reads=[b_t1[1], bsrc], writes=[b_t1[1]])
                        P.op("act", ACT(dst[:, ts], ang2, AF.Sin, scale=0.999999), reads=[b_t1[1]], writes=[b_cs])
                rot_i = 0
                for h in range(4):
                    ring_align2()
                    wcol = lambda base, e: win_d[l, :, base + h * 256 + e * 128: base + h * 256 + (e + 1) * 128]
                    s_q = [wload(wcol(5120, e), 8) for e in range(2)]
                    s_k = [wload(wcol(6144, e), 8) for e in range(2)]
                    s_v = [wload(wcol(7168, e), 8) for e in range(2)]
                    s_z = [wload(wcol(8192, e), 8) for e in range(2)]
                    assert s_v[1] == s_v[0] + 1 and s_z[1] == s_z[0] + 1
                    for hf in range(S // HS):
                        for slots, dstT, bdst in ((s_q, qT, b_qT), (s_k, kT, b_kT)):
                            for tl in range(HS // 512):
                                t = hf * (HS // 512) + tl
                                ts = slice(t * 512, (t + 1) * 512)
                                tls = slice(tl * 512, (tl + 1) * 512)
                                pb = (rot_i % 2) * 2
                                rot_i += 1
                                proj_feat(slots[0], t, pb)
                                proj_feat(slots[1], t, pb + 1)
                                pA, pB = psf[pb][:, :], psf[pb + 1][:, :]
                                rd = [b_ps[pb], b_ps[pb + 1], b_cs]
                                P.op("dve", TT(t1[0], pA, cosT[:, ts], ALU.mult), reads=rd, writes=[b_t1[0]])
                                P.op("dve", TT(t1[1], pB, sinT[:, ts], ALU.mult), reads=rd, writes=[b_t1[1]])
                                P.op("dve", TT(dstT[:, 0, tls], t1[0], t1[1], ALU.subtract), reads=b_t1, writes=[bdst])
                                P.op("dve", TT(t1[0], pB, cosT[:, ts], ALU.mult), reads=rd, writes=[b_t1[0]])
                                P.op("dve", TT(t1[1], pA, sinT[:, ts], ALU.mult), reads=rd, writes=[b_t1[1]])
                                P.op("dve", TT(dstT[:, 1, tls], t1[0], t1[1], ALU.add), reads=b_t1, writes=[bdst])
                        for slots, dst, bdst, fn in ((s_v, vr, b_vr, None), (s_z, szr, b_szr, AF.Silu)):
                            for c2 in range(NCH // 2):
                                bank = 4 + (c2 % 2)
                                mm = []
                                for q in range(2):
                                    tok0 = hf * HS + (c2 * 2 + q) * 128
                                    for c in range(8):
                                        mm.append(MM(psf[bank][:, q * 256:(q + 1) * 256], hT[:, c, tok0:tok0 + 128],
                                                     wring[:, slots[0]:slots[0] + 2, c, :], start=(c == 0), stop=(c == 7), sg=True))
                                P.op("pe", mm, reads=[b_hT, b_ring[slots[0]], b_ring[slots[1]]], writes=[b_ps[bank]])
                                o_ap = dst[:, c2 * 2:c2 * 2 + 2, :]
                                i_ap = psf[bank][:, :].rearrange("p (q n) -> p q n", q=2)
                                if fn is None:
                                    evac_copy(o_ap, i_ap, [b_ps[bank]], [bdst])
                                else:
                                    P.op("act", ACT(o_ap, i_ap, AF.Silu), reads=[b_ps[bank]], writes=[bdst])
                        for c2 in range(NCH // 2):
                            tr = []
                            for q in range(2):
                                cc = c2 * 2 + q
                                for e2 in range(2):
                                    tr.append(TR(psb[:, (q * 2 + e2) * 128:(q * 2 + e2 + 1) * 128], kT[:, e2, cc * 128:(cc + 1) * 128], identb))
                            P.op("pe", tr, reads=[b_kT, b_cb], writes=[b_psb])
                            P.op("act", ACT(kd[:, c2 * 2:c2 * 2 + 2, :], psb[:, 0:512].rearrange("p (q n) -> p q n", q=2),
                                            AF.Copy, scale=kdec[:, h:h + 1]), reads=[b_psb, b_const], writes=[b_kd])
                        for cc in range(NCH):
                            gc = hf * NCH + cc
                            cs = slice(cc * 128, (cc + 1) * 128)
                            ai = gc % 2
                            P.op("pe", [MM(psf[6][:, 0:128], kT[:, 0, cs], qT[:, 0, cs], start=True, stop=False),
                                        MM(psf[6][:, 0:128], kT[:, 1, cs], qT[:, 1, cs], start=False, stop=True)],
                                 reads=[b_kT, b_qT], writes=[b_ps[6]])
                            P.op("dve", TT(attS[ai], psf[6][:, 0:128], amask[:, h, :], ALU.mult),
                                 reads=[b_ps[6], b_const], writes=[b_attS[ai]])
                            P.op("pe", MM(psf[0][:, 0:256], attS[ai], vr[:, cc, :]), reads=[b_attS[ai], b_vr], writes=[b_ps[0]])
                            if gc > 0:
                                sbi = (gc - 1) % 2
                                P.op("pe", [MM(psf[1][:, 0:256], qT[:, 0, cs], stb[sbi][:, 0, :], start=True, stop=False),
                                            MM(psf[1][:, 0:256], qT[:, 1, cs], stb[sbi][:, 1, :], start=False, stop=True)],
                                     reads=[b_qT, b_stb[sbi]], writes=[b_ps[1]])
                                P.op("act", ACT(o1, psf[0][:, 0:256], AF.Copy), reads=[b_ps[0]], writes=[b_o1])
                                P.op("dve", STT(o2, psf[1][:, 0:256], qdec[:, h:h + 1], o1, ALU.mult, ALU.add),
                                     reads=[b_ps[1], b_o1, b_const], writes=[b_o2])
                            else:
                                P.op("act", ACT(o2, psf[0][:, 0:256], AF.Copy), reads=[b_ps[0]], writes=[b_o2])
                            if gc < 15:
                                P.op("pe", [MM(psf[2][:, 0:256], kd[:, cc, 0:128], vr[:, cc, :], sg=True),
                                            MM(psf[2][:, 256:512], kd[:, cc, 128:256], vr[:, cc, :], sg=True)],
                                     reads=[b_kd, b_vr], writes=[b_ps[2]])
                                if gc == 0:
                                    P.op("dve", CP(stf, psf[2][:, :]), reads=[b_ps[2]], writes=[b_stf])
                                else:
                                    P.op("dve", STT(stf, stf, CDEC[h], psf[2][:, :], ALU.mult, ALU.add),
                                         reads=[b_ps[2], b_stf], writes=[b_stf])
                                P.op("act", ACT(stb[gc % 2], stf.rearrange("p (e n) -> p e n", e=2), AF.Copy),
                                     reads=[b_stf], writes=[b_stb[gc % 2]])
                            P.op("dve", ("bn_stats", dict(out=st6[:, 0:BN_S], in_=o2)), reads=[b_o2], writes=[b_st])
                            P.op("dve", ("bn_aggr", dict(out=mv[:, 0:BN_A], in_=st6[:, 0:BN_S])), reads=[b_st], writes=[b_st])
                            P.op("act", ACT(rs, mv[:, 1:2], AF.Sqrt, scale=1.0, bias=epsc), reads=[b_st, b_const], writes=[b_st])
                            P.op("dve", RCP(rs, rs), reads=[b_st], writes=[b_st])
                            P.op("dve", STT(nmr, mv[:, 0:1], -1.0, rs, ALU.mult, ALU.mult), reads=[b_st], writes=[b_st])
                            P.op("act", ACT(yr2, o2, AF.Identity, scale=rs, bias=nmr), reads=[b_o2, b_st], writes=[b_yr2])
                            P.op("dve", TT(yr, yr2, szr[:, cc, :], ALU.mult), reads=[b_yr2, b_szr], writes=[b_yr])
                            P.op("pe", [TR(psb[:, 512:640], yr[:, 0:128], identb), TR(psb[:, 640:768], yr[:, 128:256], identb)],
                                 reads=[b_yr, b_cb], writes=[b_psb])
                            tok0 = gc * 128
                            for q in range(2):
                                P.op("act", ACT(yret[:, 2 * h + q, tok0:tok0 + 128], psb[:, 512 + q * 128:640 + q * 128],
                                                AF.Copy, scale=gwT[:, l, 2 * h + q:2 * h + q + 1]),
                                     reads=[b_psb, b_p5], writes=[b_yret])

                P.barrier()
                mg = carve(0, 8 * S, BF16).rearrange("p (c n) -> p c n", c=8)
                sg_ = [carve(8192, 512, BF16), carve(8448, 512, BF16)]
                m1 = [carve(8704, 512), carve(9216, 512)]
                b_mg, b_sg, b_m1 = B(), [B(), B()], [B(), B()]
                for j in range(8):
                    s_ga = wload(win_d[l, :, 9216 + j * 128: 9216 + (j + 1) * 128], 8)
                    s_gr = wload(win_d[l, :, 10240 + j * 128: 10240 + (j + 1) * 128], 8)
                    s_pa = wload(wpa_d[l, :, j * 128:(j + 1) * 128], 4)
                    s_pr = wload(wpr_d[l, :, j * 128:(j + 1) * 128], 8)
                    for t in range(4):
                        ts = slice(t * 512, (t + 1) * 512)
                        proj_feat(s_ga, t, 0)
                        proj_feat(s_gr, t, 1)
                        proj_feat(s_pa, t, 2, kc=4, src=yatt, bs=b_yatt)
                        proj_feat(s_pr, t, 3, kc=8, src=yret, bs=b_yret)
                        for i in range(2):
                            P.op("act", ACT(sg_[i], psf[i][:, :], AF.Sigmoid), reads=[b_ps[i]], writes=[b_sg[i]])
                            P.op("dve", TT(m1[i], psf[2 + i][:, :], sg_[i], ALU.mult), reads=[b_ps[2 + i], b_sg[i]], writes=[b_m1[i]])
                        P.op("dve", TT(mg[:, j, ts], m1[0], m1[1], ALU.add), reads=b_m1, writes=[b_mg])
                for j2 in range(8):
                    s_o = wload(wout_d[l, :, j2 * 128:(j2 + 1) * 128], 8)
                    for t in range(4):
                        ts = slice(t * 512, (t + 1) * 512)
                        bank = 4 + (t % 2)
                        proj_feat(s_o, t, bank, kc=8, src=mg, bs=b_mg)
                        P.op("dve", STT(xT[:, j2, ts], psf[bank][:, :], modT[:, l, 16 + j2, b:b + 1], xT[:, j2, ts], ALU.mult, ALU.add),
                             reads=[b_ps[bank], b_xT, b_mod], writes=[b_xT])

            P.barrier()
            sq = [carve(0, 512), carve(512, 512)]
            b_sq = [B(), B()]
            rt = carve(1024, 512)
            b_rt = B()
            tmp1 = carve(1536, 512)
            b_tmp1 = B()
            yT = carve(2048, 4096).rearrange("p (c n) -> p c n", c=8)
            b_yT = B()
            ost = [carve(6144, 1024), carve(7168, 1024)]
            b_ost = [B(), B()]
            oi = 0
            outs = [(True, y_d)] + ([(False, xo_d)] if want_xo else [])
            for t in range(4):
                ts = slice(t * 512, (t + 1) * 512)
                stats_rstd(t, rt, b_rt, sq, b_sq)
                for normed, dst_d in outs:
                    if normed:
                        for c in range(8):
                            P.op("dve", TT(tmp1, xT[:, c, ts], rt, ALU.mult), reads=[b_xT, b_rt], writes=[b_tmp1])
                            P.op("act", ACT(yT[:, c, :], tmp1, AF.Copy, scale=fnT[:, c:c + 1]), reads=[b_tmp1, b_p3], writes=[b_yT])
                    for tb in range(4):
                        k = oi % 2
                        oi += 1
                        for half in range(2):
                            bank = half
                            tr = []
                            for q in range(4):
                                c = half * 4 + q
                                src = yT[:, c, tb * 128:(tb + 1) * 128] if normed else xT[:, c, t * 512 + tb * 128: t * 512 + (tb + 1) * 128]
                                tr.append(TR(psf[bank][:, q * 128:(q + 1) * 128], src, identf))
                            P.op("pe", tr, reads=[b_yT if normed else b_xT, b_const], writes=[b_ps[bank]])
                            evac_copy(ost[k][:, half * 512:(half + 1) * 512], psf[bank][:, :], [b_ps[bank]], [b_ost[k]])
                        tok0 = t * 512 + tb * 128
                        P.dma("sp", dst_d[b, tok0:tok0 + 128, :], ost[k], ch_out[k], reads=[b_ost[k]])
        P.final_wait("sp", ch_out)
        with nc.Block() as block:
            P.replay(block)
    return nc


_CACHE = {}


def _get_prog(depth, nseq, want_xo):
    key = (depth, nseq, want_xo)
    if key not in _CACHE:
        _CACHE[key] = build(depth, nseq, want_xo)
    return _CACHE[key]


MODE = "unfused16"


def kernel(x, c, positions, norm_w, w_ada, b_ada, w_in, ret_gn_w, w_proj_attn, w_proj_ret, w_out, final_norm_w):
    x = np.asarray(x, dtype=np.float32)
    Bt = x.shape[0]
    depth = int(np.asarray(norm_w).shape[0])
    cf, cb, _ = host_consts()
    f = lambda a: np.ascontiguousarray(np.asarray(a, dtype=np.float32))
    c = f(c)
    positions = np.ascontiguousarray(np.asarray(positions, dtype=np.int32))
    norm_w, w_ada, b_ada, w_in, ret_gn_w = f(norm_w), f(w_ada), f(b_ada), f(w_in), f(ret_gn_w)
    w_proj_attn, w_proj_ret, w_out, final_norm_w = f(w_proj_attn), f(w_proj_ret), f(w_out), f(final_norm_w)

    def launch(xin, rows, nseq, lsl, want_xo):
        d = lsl.stop - lsl.start
        nc = _get_prog(d, nseq, want_xo)
        in_maps = []
        for i in range(NCORES):
            in_maps.append({
                "x": np.ascontiguousarray(xin[i]), "c": np.ascontiguousarray(c[rows[i]]),
                "pos": np.ascontiguousarray(positions[rows[i]]),
                "norm_w": norm_w[lsl], "w_ada": w_ada[lsl], "b_ada": b_ada[lsl], "w_in": w_in[lsl], "gn_w": ret_gn_w[lsl],
                "w_pa": w_proj_attn[lsl], "w_pr": w_proj_ret[lsl], "w_out": w_out[lsl], "fnw": final_norm_w,
                "cf": cf, "cb": cb})
        res = run_bass_kernel_spmd(nc, in_maps, core_ids=list(range(NCORES)))
        y = np.stack([r["y"] for r in res.results], axis=0)
        xo = np.stack([r["xo"] for r in res.results], axis=0) if want_xo else None
        return y, xo

    out = np.empty_like(x)
    if MODE == "fused":
        nseq = Bt // NCORES
        rows = np.arange(Bt).reshape(NCORES, nseq)
        y, _ = launch(x[rows], rows, nseq, slice(0, depth), False)
        out[rows] = y
        return out
    ngrp = Bt // NCORES
    for g in range(ngrp):
        rows = (g * NCORES + np.arange(NCORES)).reshape(NCORES, 1)
        if MODE == "layers4":
            y, _ = launch(x[rows], rows, 1, slice(0, depth), False)
        else:
            cur = x[rows]
            for l in range(depth):
                y, cur = launch(cur, rows, 1, slice(l, l + 1), True)
        out[rows] = y
    return out
```

```python
import math
import numpy as np
from contextlib import ExitStack
import concourse.bass as bass
import concourse.mybir as mybir
from concourse.bass_utils import run_bass_kernel_spmd

F32 = mybir.dt.float32
BF16 = mybir.dt.bfloat16
I32 = mybir.dt.int32
AF = mybir.ActivationFunctionType
ALU = mybir.AluOpType

S = 2048
D = 1024
INW = 11264
EPS = 1e-6
NCORES = 8
NS = 10
PI = math.pi


class Buf:
    __slots__ = ("w", "r")

    def __init__(self):
        self.w = {}
        self.r = {}


class Eng:
    def __init__(self, name, sem):
        self.name = name
        self.sem = sem
        self.count = 0
        self.waited = {}
        self.items = []


class Chan:
    def __init__(self, sem):
        self.sem = sem
        self.count = 0


class Prog:
    def __init__(self, nc, sems):
        self.nc = nc
        self.free_sems = list(sems)
        self.E = {n: Eng(n, self.free_sems.pop()) for n in ("pe", "act", "dve", "pool", "sp")}
        self.chans = []

    def chan(self):
        c = Chan(self.free_sems.pop())
        self.chans.append(c)
        return c

    def _deps(self, reads, writes):
        deps = {}
        for b in reads:
            for s, v in b.w.items():
                if deps.get(s, 0) < v:
                    deps[s] = v
        for b in writes:
            for s, v in b.w.items():
                if deps.get(s, 0) < v:
                    deps[s] = v
            for s, v in b.r.items():
                if deps.get(s, 0) < v:
                    deps[s] = v
        return deps

    def _emit_waits(self, e, deps, skip_own=False):
        for s, v in deps.items():
            if skip_own and s is e.sem:
                continue
            if e.waited.get(s, 0) < v:
                e.items.append(("w", s, v))
                e.waited[s] = v

    def _commit(self, tok, reads, writes):
        s, v = tok
        for b in reads:
            if b.r.get(s, 0) < v:
                b.r[s] = v
        for b in writes:
            b.w = {s: v}
            b.r = {}

    def op(self, eng, insts, reads=(), writes=()):
        if isinstance(insts, tuple):
            insts = [insts]
        e = self.E[eng]
        self._emit_waits(e, self._deps(reads, writes), skip_own=(eng == "pe"))
        e.count += 1
        e.items.append(("o", insts, e.sem, 1))
        self._commit((e.sem, e.count), reads, writes)

    def dma(self, eng, out, in_, chan, reads=(), writes=(), nonc=False):
        e = self.E[eng]
        self._emit_waits(e, self._deps(reads, writes))
        chan.count += 16
        e.items.append(("d" if nonc else "o", [("dma_start", dict(out=out, in_=in_))], chan.sem, 16))
        self._commit((chan.sem, chan.count), reads, writes)

    def barrier(self, engs=("pe", "act", "dve", "sp")):
        deps = {}
        for n in engs:
            e = self.E[n]
            if e.count and n != "sp":
                deps[e.sem] = e.count
        for n in engs:
            self._emit_waits(self.E[n], deps)

    def final_wait(self, eng, chans):
        e = self.E[eng]
        self._emit_waits(e, {c.sem: c.count for c in chans if c.count})

    def replay(self, block):
        nc = self.nc

        def run(e, engobj):
            for it in e.items:
                if it[0] == "w":
                    engobj.wait_ge(it[1], it[2])
                elif it[0] == "d":
                    with nc.allow_non_contiguous_dma(reason="tiny parameter layout loads"):
                        for name, kw in it[1]:
                            inst = getattr(engobj, name)(**kw)
                    inst.then_inc(it[2], it[3])
                else:
                    for name, kw in it[1]:
                        inst = getattr(engobj, name)(**kw)
                    inst.then_inc(it[2], it[3])

        @block.tensor
        def _(eng):
            run(self.E["pe"], eng)

        @block.scalar
        def _(eng):
            run(self.E["act"], eng)

        @block.vector
        def _(eng):
            run(self.E["dve"], eng)

        @block.gpsimd
        def _(eng):
            run(self.E["pool"], eng)

        @block.sync
        def _(eng):
            run(self.E["sp"], eng)


def host_consts():
    ident = np.eye(128, dtype=np.float32)
    ones = np.ones((128, 128), dtype=np.float32)
    j = np.arange(128)[:, None]
    i = np.arange(128)[None, :]
    mask = np.zeros((128, 256), dtype=np.float32)
    mask[:, 0:128] = np.where(i >= j, 0.0, -30000.0)
    mask[:, 128:256] = np.where(i <= j, 0.0, -30000.0)
    gam = 1.0 - np.exp2(-5.0 - np.arange(4, dtype=np.float64))
    lg = np.log(gam)
    m = np.arange(128)[:, None]
    n = np.arange(128)[None, :]
    amask = np.zeros((128, 4, 128), dtype=np.float32)
    qdec = np.zeros((128, 4), dtype=np.float32)
    kdec = np.zeros((128, 4), dtype=np.float32)
    for h in range(4):
        amask[:, h, :] = np.where(n >= m, np.exp(lg[h] * np.maximum(n - m, 0)), 0.0) / 16.0
        qdec[:, h] = np.exp(lg[h] * (np.arange(128) + 1.0))
        kdec[:, h] = np.exp(lg[h] * (127.0 - np.arange(128))) / 16.0
    cdec = [float(np.exp(lg[h] * 128.0)) for h in range(4)]
    theta = (np.float32(10000.0) ** (-(np.arange(128, dtype=np.float32) / np.float32(128.0)))).astype(np.float32)[:, None]
    cf = np.concatenate([ident, ones, amask.reshape(128, 512), qdec, kdec, theta,
                         np.full((128, 1), EPS, np.float32)], axis=1)
    cb = np.concatenate([ident, ones, mask], axis=1)
    return np.ascontiguousarray(cf), np.ascontiguousarray(cb), cdec


CF_W = 128 + 128 + 512 + 4 + 4 + 1 + 1
_, _, CDEC = host_consts()


def MM(out, lhsT, rhs, start=True, stop=True, sg=False):
    kw = dict(out=out, lhsT=lhsT, rhs=rhs, start=start, stop=stop)
    if sg:
        kw["skip_group_check"] = True
    return ("matmul", kw)


def TR(out, in_, ident):
    return ("transpose", dict(out=out, in_=in_, identity=ident))


def ACT(out, in_, func, scale=None, bias=None):
    kw = dict(out=out, in_=in_, func=func)
    if scale is not None:
        kw["scale"] = scale
    if bias is not None:
        kw["bias"] = bias
    return ("activation", kw)


def TT(out, in0, in1, op):
    return ("tensor_tensor", dict(out=out, in0=in0, in1=in1, op=op))


def TS(out, in0, s1, s2, op0, op1=None):
    kw = dict(out=out, in0=in0, scalar1=s1, scalar2=s2, op0=op0)
    if op1 is not None:
        kw["op1"] = op1
    return ("tensor_scalar", kw)


def STT(out, in0, scalar, in1, op0, op1):
    return ("scalar_tensor_tensor", dict(out=out, in0=in0, scalar=scalar, in1=in1, op0=op0, op1=op1))


def CP(out, in_):
    return ("tensor_copy", dict(out=out, in_=in_))


def RCP(out, in_):
    return ("reciprocal", dict(out=out, in_=in_))


def build(depth, nseq, want_xo):
    nc = bass.Bass("TRN2", target_bir_lowering=False)
    dt_ = nc.dram_tensor
    x_d = dt_("x", [nseq, S, D], F32, kind="ExternalInput").ap()
    c_d = dt_("c", [nseq, D], F32, kind="ExternalInput").ap()
    pos_d = dt_("pos", [nseq, S], I32, kind="ExternalInput").ap()
    nw_d = dt_("norm_w", [depth, D], F32, kind="ExternalInput").ap()
    wada_d = dt_("w_ada", [depth, D, 3 * D], F32, kind="ExternalInput").ap()
    bada_d = dt_("b_ada", [depth, 3 * D], F32, kind="ExternalInput").ap()
    win_d = dt_("w_in", [depth, D, INW], F32, kind="ExternalInput").ap()
    gnw_d = dt_("gn_w", [depth, D], F32, kind="ExternalInput").ap()
    wpa_d = dt_("w_pa", [depth, 512, D], F32, kind="ExternalInput").ap()
    wpr_d = dt_("w_pr", [depth, D, D], F32, kind="ExternalInput").ap()
    wout_d = dt_("w_out", [depth, D, D], F32, kind="ExternalInput").ap()
    fnw_d = dt_("fnw", [D], F32, kind="ExternalInput").ap()
    cf_d = dt_("cf", [128, CF_W], F32, kind="ExternalInput").ap()
    cb_d = dt_("cb", [128, 512], F32, kind="ExternalInput").ap()
    y_d = dt_("y", [nseq, S, D], F32, kind="ExternalOutput").ap()
    xo_d = dt_("xo", [nseq, S, D], F32, kind="ExternalOutput").ap() if want_xo else None

    BN_S = nc.vector.BN_STATS_DIM
    BN_A = nc.vector.BN_AGGR_DIM
    es = ExitStack()
    with es:
        sems = [es.enter_context(nc.semaphore(f"s{i}")) for i in range(64)]
        sb = lambda name, shape, dt: es.enter_context(nc.sbuf_tensor("sb_" + name, shape, dt))
        xT = sb("xT", [128, 8, S], F32)
        hT = sb("hT", [128, 8, S], BF16)
        yatt = sb("yatt", [128, 4, S], BF16)
        yret = sb("yret", [128, 8, S], BF16)
        wring = sb("wring", [128, NS, 8, 128], BF16)
        cf = sb("cf", [128, CF_W], F32)
        cb = sb("cb", [128, 512], BF16)
        nwT = sb("nwT", [128, depth, 8], F32)
        gwT = sb("gwT", [128, depth, 8], F32)
        baT = sb("baT", [128, depth, 24], F32)
        fnT = sb("fnT", [128, 8], F32)
        cA = sb("cA", [128, 8, nseq], F32)
        modT = sb("modT", [128, depth, 24, nseq], F32)
        aT = sb("aT", [128, depth, 8, nseq], F32)
        AR = 9472
        arena = sb("arena", [128, AR], F32)
        psf = [es.enter_context(nc.psum_tensor(f"ps{i}", [128, 512], F32)) for i in range(7)]
        psb = es.enter_context(nc.psum_tensor("psb", [128, 1024], BF16))

        identf = cf[:, 0:128]
        onesf = cf[:, 128:256]
        amask = cf[:, 256:768].rearrange("p (h n) -> p h n", h=4)
        qdec = cf[:, 768:772]
        kdec = cf[:, 772:776]
        theta = cf[:, 776:777]
        epsc = cf[:, 777:778]
        identb = cb[:, 0:128]
        onesb = cb[:, 128:256]
        maskb = cb[:, 256:512]

        P = Prog(nc, sems)
        B = Buf
        b_xT, b_hT, b_yatt, b_yret = B(), B(), B(), B()
        b_const, b_cb, b_small = B(), B(), B()
        b_ps = [B() for _ in range(7)]
        b_psb = B()
        b_xst = [B(), B()]
        ch_xst = [P.chan(), P.chan()]
        ch_cf, ch_cb, ch_posi = P.chan(), P.chan(), P.chan()
        ch_out = [P.chan(), P.chan()]
        b_ring = [B() for _ in range(NS)]
        ch_ring = [P.chan() for _ in range(NS)]
        ring_pos = [0]

        def carve(off, n, dt=F32):
            if dt == F32:
                assert off + n <= AR
                return arena[:, off:off + n]
            assert off + n // 2 <= AR
            return arena[:, off:off + n // 2].bitcast(BF16)

        def wload(src_ap, kc):
            slot = ring_pos[0] % NS
            ring_pos[0] += 1
            P.dma("pool", wring[:, slot, 0:kc, :], src_ap.rearrange("(c p) n -> p c n", p=128),
                  ch_ring[slot], writes=[b_ring[slot]])
            return slot

        def ring_align2():
            if ring_pos[0] % 2:
                ring_pos[0] += 1

        P.dma("sp", cf[:], cf_d[:, :], ch_cf, writes=[b_const])
        P.dma("pool", cb[:], cb_d[:, :], ch_cb, writes=[b_cb])
        b_p1, b_p2, b_p3, b_p4, b_p5 = B(), B(), B(), B(), B()
        ch_p = [P.chan() for _ in range(5)]
        for l in range(depth):
            P.dma("sp", nwT[:, l, :], nw_d[l].rearrange("(c p) -> p c", p=128), ch_p[0], writes=[b_p1], nonc=True)
            P.dma("sp", baT[:, l, :], bada_d[l].rearrange("(c p) -> p c", p=128), ch_p[1], writes=[b_p2], nonc=True)
            P.dma("sp", gwT[:, l, :], gnw_d[l].rearrange("(c p) -> p c", p=128), ch_p[4], writes=[b_p5], nonc=True)
        P.dma("sp", fnT[:], fnw_d.rearrange("(c p) -> p c", p=128), ch_p[2], writes=[b_p3], nonc=True)
        for bb in range(nseq):
            P.dma("sp", cA[:, :, bb], c_d[bb].rearrange("(c p) -> p c", p=128), ch_p[3], writes=[b_p4], nonc=True)
        P.op("act", ACT(cA[:], cA[:], AF.Silu), reads=[b_p4], writes=[b_p4])

        wst = [carve(i * 4096, 4096).rearrange("p (c n) -> p c n", c=8) for i in range(2)]
        b_wst = [B(), B()]
        ch_wst = [P.chan(), P.chan()]
        b_mod = B()
        it = 0
        for l in range(depth):
            for jg in range(6):
                k = it % 2
                it += 1
                P.dma("sp", wst[k], wada_d[l, :, jg * 512:(jg + 1) * 512].rearrange("(c p) n -> p c n", p=128),
                      ch_wst[k], writes=[b_wst[k]])
                mm = []
                for j4 in range(4):
                    for kc in range(8):
                        mm.append(MM(psf[0][:, j4 * nseq:(j4 + 1) * nseq], wst[k][:, kc, j4 * 128:(j4 + 1) * 128],
                                     cA[:, kc, :], start=(kc == 0), stop=(kc == 7)))
                P.op("pe", mm, reads=[b_wst[k], b_p4], writes=[b_ps[0]])
                P.op("dve", TT(modT[:, l, jg * 4:(jg + 1) * 4, :],
                               psf[0][:, 0:4 * nseq].rearrange("p (j b) -> p j b", j=4),
                               baT[:, l, jg * 4:(jg + 1) * 4].unsqueeze(2).broadcast_to([128, 4, nseq]), ALU.add),
                     reads=[b_ps[0], b_p2], writes=[b_mod])
        for l in range(depth):
            P.op("dve", STT(aT[:, l, :, :], modT[:, l, 8:16, :], 1.0,
                            nwT[:, l, :].unsqueeze(2).broadcast_to([128, 8, nseq]), ALU.add, ALU.mult),
                 reads=[b_mod, b_p1], writes=[b_small])

        def stats_rstd(t, rt, b_rt, sq, b_sq):
            ts = slice(t * 512, (t + 1) * 512)
            for c in range(8):
                k = c % 2
                P.op("act", ACT(sq[k], xT[:, c, ts], AF.Square), reads=[b_xT], writes=[b_sq[k]])
                P.op("pe", MM(psf[6][:, :], onesf, sq[k], start=(c == 0), stop=(c == 7)),
                     reads=[b_sq[k], b_const], writes=[b_ps[6]])
            P.op("act", ACT(rt, psf[6][:, :], AF.Sqrt, scale=1.0 / D, bias=epsc), reads=[b_ps[6], b_const], writes=[b_rt])
            P.op("dve", RCP(rt, rt), reads=[b_rt], writes=[b_rt])

        def proj_feat(slot, t, bank, kc=8, src=None, bs=None):
            src = hT if src is None else src
            bs = b_hT if bs is None else bs
            mm = [MM(psf[bank][:, :], wring[:, slot, c, :], src[:, c, t * 512:(t + 1) * 512],
                     start=(c == 0), stop=(c == kc - 1)) for c in range(kc)]
            P.op("pe", mm, reads=[b_ring[slot], bs], writes=[b_ps[bank]])

        evac_flip = [0]

        def evac_copy(out, in_, reads, writes, scale=None):
            evac_flip[0] ^= 1
            if evac_flip[0]:
                P.op("act", ACT(out, in_, AF.Copy, scale=scale), reads=reads, writes=writes)
            elif scale is None:
                P.op("dve", CP(out, in_), reads=reads, writes=writes)
            else:
                P.op("dve", TS(out, in_, scale, None, ALU.mult), reads=reads, writes=writes)

        for b in range(nseq):
            P.barrier()
            xst = [carve(0, 1024), carve(1024, 1024)]
            for tt in range(16):
                k = tt % 2
                P.dma("sp", xst[k], x_d[b, tt * 128:(tt + 1) * 128, :], ch_xst[k], writes=[b_xst[k]])
                for half in range(2):
                    bank = half
                    tr = [TR(psf[bank][:, q * 128:(q + 1) * 128], xst[k][:, (half * 4 + q) * 128:(half * 4 + q + 1) * 128], identf)
                          for q in range(4)]
                    P.op("pe", tr, reads=[b_xst[k], b_const], writes=[b_ps[bank]])
                    evac_copy(xT[:, half * 4:half * 4 + 4, tt * 128:(tt + 1) * 128],
                              psf[bank][:, :].rearrange("p (q n) -> p q n", q=4), [b_ps[bank]], [b_xT])

            for l in range(depth):
                P.barrier()
                sq = [carve(0, 512), carve(512, 512)]
                b_sq = [B(), B()]
                rt = carve(1024, 512)
                b_rt = B()
                tmp = [carve(1536, 512), carve(2048, 512)]
                b_tmp = [B(), B()]
                for t in range(4):
                    ts = slice(t * 512, (t + 1) * 512)
                    stats_rstd(t, rt, b_rt, sq, b_sq)
                    for c in range(8):
                        k = c % 2
                        P.op("dve", TT(tmp[k], xT[:, c, ts], rt, ALU.mult), reads=[b_xT, b_rt], writes=[b_tmp[k]])
                        P.op("act", ACT(hT[:, c, ts], tmp[k], AF.Identity, scale=aT[:, l, c, b:b + 1], bias=modT[:, l, c, b:b + 1]),
                             reads=[b_tmp[k], b_small, b_mod], writes=[b_hT])

                P.barrier()
                sza = carve(0, S, BF16)
                Uacc = carve(1024, S)
                Lacc = carve(3072, S)
                pT = [carve(5120, 512, BF16), carve(5376, 512, BF16)]
                rl = carve(5632, 512)
                qkv = []
                for i in range(2):
                    qkv.append(dict(q=yret[:, 4 * i, :], k=yret[:, 4 * i + 1, :], vT=yret[:, 4 * i + 2, :],
                                    v=yret[:, 4 * i + 3, :].rearrange("p (k n) -> p k n", k=16),
                                    bq=B(), bk=B(), bvT=B(), bv=B()))
                b_pT = [B(), B()]
                b_sza, b_U, b_L, b_rl = B(), B(), B(), B()
                gi = 0
                sc_i = 0
                for s in range(4):
                    slot = wload(win_d[l, :, 4608 + s * 128: 4608 + (s + 1) * 128], 8)
                    for t in range(4):
                        bank = t % 2
                        proj_feat(slot, t, bank)
                        P.op("act", ACT(sza[:, t * 512:(t + 1) * 512], psf[bank][:, :], AF.Silu),
                             reads=[b_ps[bank]], writes=[b_sza])
                    for g, dil in enumerate((1, 4, 16)):
                        Q = qkv[gi % 2]
                        gi += 1
                        L = S // dil
                        CL = L // 128
                        for name, off, bb, scale in (("q", 0, Q["bq"], 128 ** -0.5), ("k", 1536, Q["bk"], None),
                                                     ("vT", 3072, Q["bvT"], None)):
                            c0 = off + g * 512 + s * 128
                            slot = wload(win_d[l, :, c0:c0 + 128], 8)
                            dst = Q[name]
                            for t in range(4):
                                bank = t % 2
                                proj_feat(slot, t, bank)
                                m0 = t * 512 // dil
                                mw = 512 // dil
                                if dil == 1:
                                    o_ap = dst[:, t * 512:(t + 1) * 512]
                                    i_ap = psf[bank][:, :]
                                else:
                                    o_ap = dst.rearrange("p (r m) -> p r m", r=dil)[:, :, m0:m0 + mw]
                                    i_ap = psf[bank][:, :].rearrange("p (m r) -> p r m", r=dil)
                                evac_copy(o_ap, i_ap, [b_ps[bank]], [bb], scale=scale)
                        for q4 in range(4):
                            tr = [TR(psb[:, q * 128:(q + 1) * 128], Q["vT"][:, (q4 * 4 + q) * 128:(q4 * 4 + q + 1) * 128], identb)
                                  for q in range(4)]
                            P.op("pe", tr, reads=[Q["bvT"], b_cb], writes=[b_psb])
                            evac_copy(Q["v"][:, q4 * 4:q4 * 4 + 4, :], psb[:, 0:512].rearrange("p (q n) -> p q n", q=4),
                                      [b_psb], [Q["bv"]])
                        for Bk in range(4):
                            q_lo = 4 * Bk
                            kb_lo = q_lo - 1 if (q_lo % CL) != 0 else q_lo
                            first = True
                            for kb in range(kb_lo, q_lo + 4):
                                qbs = []
                                if kb >= q_lo:
                                    qbs.append(kb)
                                if (kb + 1) % CL != 0 and kb + 1 <= q_lo + 3:
                                    qbs.append(kb + 1)
                                n = 128 * len(qbs)
                                q0 = qbs[0]
                                m_lo = 0 if qbs[0] == kb else 128
                                sb_i = sc_i % 2
                                sc_i += 1
                                bankS = 2 + sb_i
                                P.op("pe", [MM(psf[bankS][:, 0:n], Q["k"][:, kb * 128:(kb + 1) * 128],
                                               Q["q"][:, q0 * 128:q0 * 128 + n], start=True, stop=False),
                                            MM(psf[bankS][:, 0:n], identb, maskb[:, m_lo:m_lo + n], start=False, stop=True)],
                                     reads=[Q["bq"], Q["bk"], b_cb], writes=[b_ps[bankS]])
                                P.op("act", ACT(pT[sb_i][:, 0:n], psf[bankS][:, 0:n], AF.Exp),
                                     reads=[b_ps[bankS]], writes=[b_pT[sb_i]])
                                last = (kb == q_lo + 3)
                                col = (q0 - q_lo) * 128
                                P.op("pe", [MM(psf[4][:, col:col + n], Q["v"][:, kb, :], pT[sb_i][:, 0:n], start=first, stop=last, sg=True),
                                            MM(psf[5][:, col:col + n], onesb, pT[sb_i][:, 0:n], start=first, stop=last, sg=True)],
                                     reads=[Q["bv"], b_pT[sb_i], b_cb], writes=[b_ps[4], b_ps[5]])
                                first = False
                            for acc, bacc, bank in ((Uacc, b_U, 4), (Lacc, b_L, 5)):
                                if dil == 1:
                                    o_ap = acc[:, Bk * 512:(Bk + 1) * 512]
                                    i_ap = psf[bank][:, :]
                                elif dil == 4:
                                    o_ap = acc.rearrange("p (m r) -> p r m", r=4)[:, Bk, :]
                                    i_ap = psf[bank][:, :]
                                else:
                                    o_ap = acc.rearrange("p (m r) -> p r m", r=16)[:, 4 * Bk:4 * Bk + 4, :]
                                    i_ap = psf[bank][:, :].rearrange("p (r m) -> p r m", r=4)
                                if g == 0:
                                    P.op("dve", CP(o_ap, i_ap), reads=[b_ps[bank]], writes=[bacc])
                                else:
                                    P.op("dve", TT(o_ap, i_ap, o_ap, ALU.add), reads=[b_ps[bank], bacc], writes=[bacc])
                    for t in range(4):
                        ts = slice(t * 512, (t + 1) * 512)
                        P.op("dve", RCP(rl, Lacc[:, ts]), reads=[b_L], writes=[b_rl])
                        P.op("dve", TT(rl, Uacc[:, ts], rl, ALU.mult), reads=[b_U, b_rl], writes=[b_rl])
                        P.op("dve", TT(yatt[:, s, ts], rl, sza[:, ts], ALU.mult), reads=[b_rl, b_sza], writes=[b_yatt])

                P.barrier()
                HS = 512
                NCH = HS // 128
                cosT = carve(0, S)
                sinT = carve(2048, S)
                qT = carve(4096, 2 * HS, BF16).rearrange("p (e n) -> p e n", e=2)
                kT = carve(4608, 2 * HS, BF16).rearrange("p (e n) -> p e n", e=2)
                kd = carve(5120, NCH * 256, BF16).rearrange("p (c n) -> p c n", c=NCH)
                vr = carve(5632, NCH * 256, BF16).rearrange("p (c n) -> p c n", c=NCH)
                szr = carve(6144, NCH * 256, BF16).rearrange("p (c n) -> p c n", c=NCH)
                t1 = [carve(6656, 512), carve(7168, 512)]
                stf = carve(7680, 512)
                stb = [carve(8192, 512, BF16).rearrange("p (e n) -> p e n", e=2),
                       carve(8448, 512, BF16).rearrange("p (e n) -> p e n", e=2)]
                attS = [carve(8704, 128, BF16), carve(8768, 128, BF16)]
                o2 = carve(8832, 256)
                yr = carve(8832, 256, BF16)
                yr2 = carve(9088, 256)
                o1 = yr2
                st6 = carve(9344, 8)
                mv = carve(9352, 4)
                rs = carve(9356, 1)
                nmr = carve(9358, 1)
                b_cs = B()
                b_qT, b_kT, b_kd, b_vr, b_szr = B(), B(), B(), B(), B()
                b_t1 = [B(), B()]
                b_stf = B()
                b_stb = [B(), B()]
                b_attS = [B(), B()]
                b_o2, b_yr2, b_st = B(), B(), B()
                b_o1, b_yr = b_yr2, b_o2
                posi = stf.bitcast(I32)
                ang, ang2 = t1[0], t1[1]
                for t in range(4):
                    ts = slice(t * 512, (t + 1) * 512)
                    P.dma("sp", posi, pos_d[b:b + 1, ts].broadcast_to([128, 512]), ch_posi, writes=[b_stf])
                    P.op("dve", CP(ang, posi), reads=[b_stf], writes=[b_t1[0]])
                    P.op("dve", TS(ang, ang, theta, None, ALU.mult), reads=[b_t1[0], b_const], writes=[b_t1[0]])
                    MAGIC = 12582912.0
                    for dst, use_shift in ((sinT, False), (cosT, True)):
                        srcang = ang
                        bsrc = b_t1[0]
                        if use_shift:
                            P.op("dve", TS(stf, ang, PI / 2, None, ALU.add), reads=[b_t1[0]], writes=[b_stf])
                            srcang = stf
                            bsrc = b_stf
                        P.op("dve", TS(ang2, srcang, 1.0 / (2 * PI), MAGIC, ALU.mult, ALU.add), reads=[bsrc], writes=[b_t1[1]])
                        P.op("dve", TS(ang2, ang2, -MAGIC, None, ALU.add), reads=[b_t1[1]], writes=[b_t1[1]])
                        P.op("dve", STT(ang2, ang2, -2 * PI, srcang, ALU.mult, ALU.add), reads=[b_t1[1], bsrc], writes=[b_t1[1]])
                        P.op("act", ACT(dst[:, ts], ang2, AF.Sin, scale=0.999999), reads=[b_t1[1]], writes=[b_cs])
                rot_i = 0
                for h in range(4):
                    ring_align2()
                    wcol = lambda base, e: win_d[l, :, base + h * 256 + e * 128: base + h * 256 + (e + 1) * 128]
                    s_q = [wload(wcol(5120, e), 8) for e in range(2)]
                    s_k = [wload(wcol(6144, e), 8) for e in range(2)]
                    s_v = [wload(wcol(7168, e), 8) for e in range(2)]
                    s_z = [wload(wcol(8192, e), 8) for e in range(2)]
                    assert s_v[1] == s_v[0] + 1 and s_z[1] == s_z[0] + 1
                    for hf in range(S // HS):
                        for slots, dstT, bdst in ((s_q, qT, b_qT), (s_k, kT, b_kT)):
                            for tl in range(HS // 512):
                                t = hf * (HS // 512) + tl
                                ts = slice(t * 512, (t + 1) * 512)
                                tls = slice(tl * 512, (tl + 1) * 512)
                                pb = (rot_i % 2) * 2
                                rot_i += 1
                                proj_feat(slots[0], t, pb)
                                proj_feat(slots[1], t, pb + 1)
                                pA, pB = psf[pb][:, :], psf[pb + 1][:, :]
                                rd = [b_ps[pb], b_ps[pb + 1], b_cs]
                                P.op("dve", TT(t1[0], pA, cosT[:, ts], ALU.mult), reads=rd, writes=[b_t1[0]])
                                P.op("dve", TT(t1[1], pB, sinT[:, ts], ALU.mult), reads=rd, writes=[b_t1[1]])
                                P.op("dve", TT(dstT[:, 0, tls], t1[0], t1[1], ALU.subtract), reads=b_t1, writes=[bdst])
                                P.op("dve", TT(t1[0], pB, cosT[:, ts], ALU.mult), reads=rd, writes=[b_t1[0]])
                                P.op("dve", TT(t1[1], pA, sinT[:, ts], ALU.mult), reads=rd, writes=[b_t1[1]])
                                P.op("dve", TT(dstT[:, 1, tls], t1[0], t1[1], ALU.add), reads=b_t1, writes=[bdst])
                        for slots, dst, bdst, fn in ((s_v, vr, b_vr, None), (s_z, szr, b_szr, AF.Silu)):
                            for c2 in range(NCH // 2):
                                bank = 4 + (c2 % 2)
                                mm = []
                                for q in range(2):
                                    tok0 = hf * HS + (c2 * 2 + q) * 128
                                    for c in range(8):
                                        mm.append(MM(psf[bank][:, q * 256:(q + 1) * 256], hT[:, c, tok0:tok0 + 128],
                                                     wring[:, slots[0]:slots[0] + 2, c, :], start=(c == 0), stop=(c == 7), sg=True))
                                P.op("pe", mm, reads=[b_hT, b_ring[slots[0]], b_ring[slots[1]]], writes=[b_ps[bank]])
                                o_ap = dst[:, c2 * 2:c2 * 2 + 2, :]
                                i_ap = psf[bank][:, :].rearrange("p (q n) -> p q n", q=2)
                                if fn is None:
                                    evac_copy(o_ap, i_ap, [b_ps[bank]], [bdst])
                                else:
                                    P.op("act", ACT(o_ap, i_ap, AF.Silu), reads=[b_ps[bank]], writes=[bdst])
                        for c2 in range(NCH // 2):
                            tr = []
                            for q in range(2):
                                cc = c2 * 2 + q
                                for e2 in range(2):
                                    tr.append(TR(psb[:, (q * 2 + e2) * 128:(q * 2 + e2 + 1) * 128], kT[:, e2, cc * 128:(cc + 1) * 128], identb))
                            P.op("pe", tr, reads=[b_kT, b_cb], writes=[b_psb])
                            P.op("act", ACT(kd[:, c2 * 2:c2 * 2 + 2, :], psb[:, 0:512].rearrange("p (q n) -> p q n", q=2),
                                            AF.Copy, scale=kdec[:, h:h + 1]), reads=[b_psb, b_const], writes=[b_kd])
                        for cc in range(NCH):
                            gc = hf * NCH + cc
                            cs = slice(cc * 128, (cc + 1) * 128)
                            ai = gc % 2
                            P.op("pe", [MM(psf[6][:, 0:128], kT[:, 0, cs], qT[:, 0, cs], start=True, stop=False),
                                        MM(psf[6][:, 0:128], kT[:, 1, cs], qT[:, 1, cs], start=False, stop=True)],
                                 reads=[b_kT, b_qT], writes=[b_ps[6]])
                            P.op("dve", TT(attS[ai], psf[6][:, 0:128], amask[:, h, :], ALU.mult),
                                 reads=[b_ps[6], b_const], writes=[b_attS[ai]])
                            P.op("pe", MM(psf[0][:, 0:256], attS[ai], vr[:, cc, :]), reads=[b_attS[ai], b_vr], writes=[b_ps[0]])
                            if gc > 0:
                                sbi = (gc - 1) % 2
                                P.op("pe", [MM(psf[1][:, 0:256], qT[:, 0, cs], stb[sbi][:, 0, :], start=True, stop=False),
                                            MM(psf[1][:, 0:256], qT[:, 1, cs], stb[sbi][:, 1, :], start=False, stop=True)],
                                     reads=[b_qT, b_stb[sbi]], writes=[b_ps[1]])
                                P.op("act", ACT(o1, psf[0][:, 0:256], AF.Copy), reads=[b_ps[0]], writes=[b_o1])
                                P.op("dve", STT(o2, psf[1][:, 0:256], qdec[:, h:h + 1], o1, ALU.mult, ALU.add),
                                     reads=[b_ps[1], b_o1, b_const], writes=[b_o2])
                            else:
                                P.op("act", ACT(o2, psf[0][:, 0:256], AF.Copy), reads=[b_ps[0]], writes=[b_o2])
                            if gc < 15:
                                P.op("pe", [MM(psf[2][:, 0:256], kd[:, cc, 0:128], vr[:, cc, :], sg=True),
                                            MM(psf[2][:, 256:512], kd[:, cc, 128:256], vr[:, cc, :], sg=True)],
                                     reads=[b_kd, b_vr], writes=[b_ps[2]])
                                if gc == 0:
                                    P.op("dve", CP(stf, psf[2][:, :]), reads=[b_ps[2]], writes=[b_stf])
                                else:
                                    P.op("dve", STT(stf, stf, CDEC[h], psf[2][:, :], ALU.mult, ALU.add),
                                         reads=[b_ps[2], b_stf], writes=[b_stf])
                                P.op("act", ACT(stb[gc % 2], stf.rearrange("p (e n) -> p e n", e=2), AF.Copy),
                                     reads=[b_stf], writes=[b_stb[gc % 2]])
                            P.op("dve", ("bn_stats", dict(out=st6[:, 0:BN_S], in_=o2)), reads=[b_o2], writes=[b_st])
                            P.op("dve", ("bn_aggr", dict(out=mv[:, 0:BN_A], in_=st6[:, 0:BN_S])), reads=[b_st], writes=[b_st])
                            P.op("act", ACT(rs, mv[:, 1:2], AF.Sqrt, scale=1.0, bias=epsc), reads=[b_st, b_const], writes=[b_st])
                            P.op("dve", RCP(rs, rs), reads=[b_st], writes=[b_st])
                            P.op("dve", STT(nmr, mv[:, 0:1], -1.0, rs, ALU.mult, ALU.mult), reads=[b_st], writes=[b_st])
                            P.op("act", ACT(yr2, o2, AF.Identity, scale=rs, bias=nmr), reads=[b_o2, b_st], writes=[b_yr2])
                            P.op("dve", TT(yr, yr2, szr[:, cc, :], ALU.mult), reads=[b_yr2, b_szr], writes=[b_yr])
                            P.op("pe", [TR(psb[:, 512:640], yr[:, 0:128], identb), TR(psb[:, 640:768], yr[:, 128:256], identb)],
                                 reads=[b_yr, b_cb], writes=[b_psb])
                            tok0 = gc * 128
                            for q in range(2):
                                P.op("act", ACT(yret[:, 2 * h + q, tok0:tok0 + 128], psb[:, 512 + q * 128:640 + q * 128],
                                                AF.Copy, scale=gwT[:, l, 2 * h + q:2 * h + q + 1]),
                                     reads=[b_psb, b_p5], writes=[b_yret])

                P.barrier()
                mg = carve(0, 8 * S, BF16).rearrange("p (c n) -> p c n", c=8)
                sg1 = carve(8192, 512, BF16)
                sg_ = [sg1, sg1]
                m1 = [carve(8448, 512), carve(8960, 512)]
                bsg1 = B()
                b_mg, b_sg, b_m1 = B(), [bsg1, bsg1], [B(), B()]
                for j in range(8):
                    s_ga = wload(win_d[l, :, 9216 + j * 128: 9216 + (j + 1) * 128], 8)
                    s_gr = wload(win_d[l, :, 10240 + j * 128: 10240 + (j + 1) * 128], 8)
                    s_pa = wload(wpa_d[l, :, j * 128:(j + 1) * 128], 4)
                    s_pr = wload(wpr_d[l, :, j * 128:(j + 1) * 128], 8)
                    for t in range(4):
                        ts = slice(t * 512, (t + 1) * 512)
                        proj_feat(s_ga, t, 0)
                        proj_feat(s_gr, t, 1)
                        proj_feat(s_pa, t, 2, kc=4, src=yatt, bs=b_yatt)
                        proj_feat(s_pr, t, 3, kc=8, src=yret, bs=b_yret)
                        for i in range(2):
                            P.op("act", ACT(sg_[i], psf[i][:, :], AF.Sigmoid), reads=[b_ps[i]], writes=[b_sg[i]])
                            P.op("dve", TT(m1[i], psf[2 + i][:, :], sg_[i], ALU.mult), reads=[b_ps[2 + i], b_sg[i]], writes=[b_m1[i]])
                        P.op("dve", TT(mg[:, j, ts], m1[0], m1[1], ALU.add), reads=b_m1, writes=[b_mg])
                for j2 in range(8):
                    s_o = wload(wout_d[l, :, j2 * 128:(j2 + 1) * 128], 8)
                    for t in range(4):
                        ts = slice(t * 512, (t + 1) * 512)
                        bank = 4 + (t % 2)
                        proj_feat(s_o, t, bank, kc=8, src=mg, bs=b_mg)
                        P.op("dve", STT(xT[:, j2, ts], psf[bank][:, :], modT[:, l, 16 + j2, b:b + 1], xT[:, j2, ts], ALU.mult, ALU.add),
                             reads=[b_ps[bank], b_xT, b_mod], writes=[b_xT])

            P.barrier()
            sq = [carve(0, 512), carve(512, 512)]
            b_sq = [B(), B()]
            rt = carve(1024, 512)
            b_rt = B()
            tmp1 = carve(1536, 512)
            b_tmp1 = B()
            yT = carve(2048, 4096).rearrange("p (c n) -> p c n", c=8)
            b_yT = B()
            ost = [carve(6144, 1024), carve(7168, 1024)]
            b_ost = [B(), B()]
            oi = 0
            outs = [(True, y_d)] + ([(False, xo_d)] if want_xo else [])
            for t in range(4):
                ts = slice(t * 512, (t + 1) * 512)
                stats_rstd(t, rt, b_rt, sq, b_sq)
                for normed, dst_d in outs:
                    if normed:
                        for c in range(8):
                            P.op("dve", TT(tmp1, xT[:, c, ts], rt, ALU.mult), reads=[b_xT, b_rt], writes=[b_tmp1])
                            P.op("act", ACT(yT[:, c, :], tmp1, AF.Copy, scale=fnT[:, c:c + 1]), reads=[b_tmp1, b_p3], writes=[b_yT])
                    for tb in range(4):
                        k = oi % 2
                        oi += 1
                        for half in range(2):
                            bank = half
                            tr = []
                            for q in range(4):
                                c = half * 4 + q
                                src = yT[:, c, tb * 128:(tb + 1) * 128] if normed else xT[:, c, t * 512 + tb * 128: t * 512 + (tb + 1) * 128]
                                tr.append(TR(psf[bank][:, q * 128:(q + 1) * 128], src, identf))
                            P.op("pe", tr, reads=[b_yT if normed else b_xT, b_const], writes=[b_ps[bank]])
                            evac_copy(ost[k][:, half * 512:(half + 1) * 512], psf[bank][:, :], [b_ps[bank]], [b_ost[k]])
                        tok0 = t * 512 + tb * 128
                        P.dma("sp", dst_d[b, tok0:tok0 + 128, :], ost[k], ch_out[k], reads=[b_ost[k]])
        P.final_wait("sp", ch_out)
        with nc.Block() as block:
            P.replay(block)
    return nc


_CACHE = {}


def _get_prog(depth, nseq, want_xo):
    key = (depth, nseq, want_xo)
    if key not in _CACHE:
        _CACHE[key] = build(depth, nseq, want_xo)
    return _CACHE[key]


MODE = "fused"


def kernel(x, c, positions, norm_w, w_ada, b_ada, w_in, ret_gn_w, w_proj_attn, w_proj_ret, w_out, final_norm_w):
    x = np.asarray(x, dtype=np.float32)
    Bt = x.shape[0]
    depth = int(np.asarray(norm_w).shape[0])
    cf, cb, _ = host_consts()
    f = lambda a: np.ascontiguousarray(np.asarray(a, dtype=np.float32))
    c = f(c)
    positions = np.ascontiguousarray(np.asarray(positions, dtype=np.int32))
    norm_w, w_ada, b_ada, w_in, ret_gn_w = f(norm_w), f(w_ada), f(b_ada), f(w_in), f(ret_gn_w)
    w_proj_attn, w_proj_ret, w_out, final_norm_w = f(w_proj_attn), f(w_proj_ret), f(w_out), f(final_norm_w)

    def launch(xin, rows, nseq, lsl, want_xo):
        d = lsl.stop - lsl.start
        nc = _get_prog(d, nseq, want_xo)
        in_maps = []
        for i in range(NCORES):
            in_maps.append({
                "x": np.ascontiguousarray(xin[i]), "c": np.ascontiguousarray(c[rows[i]]),
                "pos": np.ascontiguousarray(positions[rows[i]]),
                "norm_w": norm_w[lsl], "w_ada": w_ada[lsl], "b_ada": b_ada[lsl], "w_in": w_in[lsl], "gn_w": ret_gn_w[lsl],
                "w_pa": w_proj_attn[lsl], "w_pr": w_proj_ret[lsl], "w_out": w_out[lsl], "fnw": final_norm_w,
                "cf": cf, "cb": cb})
        res = run_bass_kernel_spmd(nc, in_maps, core_ids=list(range(NCORES)))
        y = np.stack([r["y"] for r in res.results], axis=0)
        xo = np.stack([r["xo"] for r in res.results], axis=0) if want_xo else None
        return y, xo

    out = np.empty_like(x)
    if MODE == "fused":
        nseq = Bt // NCORES
        rows = np.arange(Bt).reshape(NCORES, nseq)
        y, _ = launch(x[rows], rows, nseq, slice(0, depth), False)
        out[rows] = y
        return out
    ngrp = Bt // NCORES
    for g in range(ngrp):
        rows = (g * NCORES + np.arange(NCORES)).reshape(NCORES, 1)
        if MODE == "layers4":
            y, _ = launch(x[rows], rows, 1, slice(0, depth), False)
        else:
            cur = x[rows]
            for l in range(depth):
                y, cur = launch(cur, rows, 1, slice(l, l + 1), True)
        out[rows] = y
    return out
```

```python
import math
import numpy as np
from contextlib import ExitStack
import concourse.bass as bass
import concourse.mybir as mybir
from concourse.bass_utils import run_bass_kernel_spmd

F32 = mybir.dt.float32
BF16 = mybir.dt.bfloat16
I32 = mybir.dt.int32
AF = mybir.ActivationFunctionType
ALU = mybir.AluOpType

S = 2048
D = 1024
INW = 11264
EPS = 1e-6
NCORES = 8
USE_POW = False
NS = 10
PI = math.pi


class Buf:
    __slots__ = ("w", "r")

    def __init__(self):
        self.w = {}
        self.r = {}


class Eng:
    def __init__(self, name, sem):
        self.name = name
        self.sem = sem
        self.count = 0
        self.waited = {}
        self.items = []


class Chan:
    def __init__(self, sem):
        self.sem = sem
        self.count = 0


class Prog:
    def __init__(self, nc, sems):
        self.nc = nc
        self.free_sems = list(sems)
        self.E = {n: Eng(n, self.free_sems.pop()) for n in ("pe", "act", "dve", "pool", "sp")}
        self.chans = []

    def chan(self):
        c = Chan(self.free_sems.pop())
        self.chans.append(c)
        return c

    def _deps(self, reads, writes):
        deps = {}
        for b in reads:
            for s, v in b.w.items():
                if deps.get(s, 0) < v:
                    deps[s] = v
        for b in writes:
            for s, v in b.w.items():
                if deps.get(s, 0) < v:
                    deps[s] = v
            for s, v in b.r.items():
                if deps.get(s, 0) < v:
                    deps[s] = v
        return deps

    def _emit_waits(self, e, deps, skip_own=False):
        for s, v in deps.items():
            if skip_own and s is e.sem:
                continue
            if e.waited.get(s, 0) < v:
                e.items.append(("w", s, v))
                e.waited[s] = v

    def _commit(self, tok, reads, writes):
        s, v = tok
        for b in reads:
            if b.r.get(s, 0) < v:
                b.r[s] = v
        for b in writes:
            b.w = {s: v}
            b.r = {}

    def op(self, eng, insts, reads=(), writes=()):
        if isinstance(insts, tuple):
            insts = [insts]
        e = self.E[eng]
        self._emit_waits(e, self._deps(reads, writes), skip_own=(eng == "pe"))
        e.count += 1
        e.items.append(("o", insts, e.sem, 1))
        self._commit((e.sem, e.count), reads, writes)

    def dma(self, eng, out, in_, chan, reads=(), writes=(), nonc=False):
        e = self.E[eng]
        self._emit_waits(e, self._deps(reads, writes))
        chan.count += 16
        e.items.append(("d" if nonc else "o", [("dma_start", dict(out=out, in_=in_))], chan.sem, 16))
        self._commit((chan.sem, chan.count), reads, writes)

    def barrier(self, engs=("pe", "act", "dve", "sp")):
        deps = {}
        for n in engs:
            e = self.E[n]
            if e.count and n != "sp":
                deps[e.sem] = e.count
        for n in engs:
            self._emit_waits(self.E[n], deps)

    def final_wait(self, eng, chans):
        e = self.E[eng]
        self._emit_waits(e, {c.sem: c.count for c in chans if c.count})

    def replay(self, block):
        nc = self.nc

        def run(e, engobj):
            for it in e.items:
                if it[0] == "w":
                    engobj.wait_ge(it[1], it[2])
                elif it[0] == "d":
                    with nc.allow_non_contiguous_dma(reason="tiny parameter layout loads"):
                        for name, kw in it[1]:
                            inst = getattr(engobj, name)(**kw)
                    inst.then_inc(it[2], it[3])
                else:
                    for name, kw in it[1]:
                        inst = getattr(engobj, name)(**kw)
                    inst.then_inc(it[2], it[3])

        @block.tensor
        def _(eng):
            run(self.E["pe"], eng)

        @block.scalar
        def _(eng):
            run(self.E["act"], eng)

        @block.vector
        def _(eng):
            run(self.E["dve"], eng)

        @block.gpsimd
        def _(eng):
            run(self.E["pool"], eng)

        @block.sync
        def _(eng):
            run(self.E["sp"], eng)


def host_consts():
    ident = np.eye(128, dtype=np.float32)
    ones = np.ones((128, 128), dtype=np.float32)
    j = np.arange(128)[:, None]
    i = np.arange(128)[None, :]
    mask = np.zeros((128, 256), dtype=np.float32)
    mask[:, 0:128] = np.where(i >= j, 0.0, -30000.0)
    mask[:, 128:256] = np.where(i <= j, 0.0, -30000.0)
    gam = 1.0 - np.exp2(-5.0 - np.arange(4, dtype=np.float64))
    lg = np.log(gam)
    m = np.arange(128)[:, None]
    n = np.arange(128)[None, :]
    amask = np.zeros((128, 4, 128), dtype=np.float32)
    qdec = np.zeros((128, 4), dtype=np.float32)
    kdec = np.zeros((128, 4), dtype=np.float32)
    for h in range(4):
        amask[:, h, :] = np.where(n >= m, np.exp(-lg[h] * (m + 1.0)), 0.0) / 16.0
        qdec[:, h] = np.exp(lg[h] * (np.arange(128) + 1.0))
        kdec[:, h] = np.exp(lg[h] * (127.0 - np.arange(128))) / 16.0
    cdec = [float(np.exp(lg[h] * 128.0)) for h in range(4)]
    theta = (np.float32(10000.0) ** (-(np.arange(128, dtype=np.float32) / np.float32(128.0)))).astype(np.float32)[:, None]
    cf = np.concatenate([ident, ones, amask.reshape(128, 512), qdec, kdec, theta,
                         np.full((128, 1), EPS, np.float32), qdec * qdec], axis=1)
    cb = np.concatenate([ident, ones, mask], axis=1)
    return np.ascontiguousarray(cf), np.ascontiguousarray(cb), cdec


CF_W = 128 + 128 + 512 + 4 + 4 + 1 + 1 + 4
_, _, CDEC = host_consts()


def MM(out, lhsT, rhs, start=True, stop=True, sg=False):
    kw = dict(out=out, lhsT=lhsT, rhs=rhs, start=start, stop=stop)
    if sg:
        kw["skip_group_check"] = True
    return ("matmul", kw)


def TR(out, in_, ident):
    return ("transpose", dict(out=out, in_=in_, identity=ident))


def ACT(out, in_, func, scale=None, bias=None):
    kw = dict(out=out, in_=in_, func=func)
    if scale is not None:
        kw["scale"] = scale
    if bias is not None:
        kw["bias"] = bias
    return ("activation", kw)


def TT(out, in0, in1, op):
    return ("tensor_tensor", dict(out=out, in0=in0, in1=in1, op=op))


def TS(out, in0, s1, s2, op0, op1=None):
    kw = dict(out=out, in0=in0, scalar1=s1, scalar2=s2, op0=op0)
    if op1 is not None:
        kw["op1"] = op1
    return ("tensor_scalar", kw)


def STT(out, in0, scalar, in1, op0, op1):
    return ("scalar_tensor_tensor", dict(out=out, in0=in0, scalar=scalar, in1=in1, op0=op0, op1=op1))


def CP(out, in_):
    return ("tensor_copy", dict(out=out, in_=in_))


def RCP(out, in_):
    return ("reciprocal", dict(out=out, in_=in_))


def build(depth, nseq, want_xo):
    nc = bass.Bass("TRN2", target_bir_lowering=False)
    dt_ = nc.dram_tensor
    x_d = dt_("x", [nseq, S, D], F32, kind="ExternalInput").ap()
    c_d = dt_("c", [nseq, D], F32, kind="ExternalInput").ap()
    pos_d = dt_("pos", [nseq, S], I32, kind="ExternalInput").ap()
    nw_d = dt_("norm_w", [depth, D], F32, kind="ExternalInput").ap()
    wada_d = dt_("w_ada", [depth, D, 3 * D], F32, kind="ExternalInput").ap()
    bada_d = dt_("b_ada", [depth, 3 * D], F32, kind="ExternalInput").ap()
    win_d = dt_("w_in", [depth, D, INW], F32, kind="ExternalInput").ap()
    gnw_d = dt_("gn_w", [depth, D], F32, kind="ExternalInput").ap()
    wpa_d = dt_("w_pa", [depth, 512, D], F32, kind="ExternalInput").ap()
    wpr_d = dt_("w_pr", [depth, D, D], F32, kind="ExternalInput").ap()
    wout_d = dt_("w_out", [depth, D, D], F32, kind="ExternalInput").ap()
    fnw_d = dt_("fnw", [D], F32, kind="ExternalInput").ap()
    cf_d = dt_("cf", [128, CF_W], F32, kind="ExternalInput").ap()
    cb_d = dt_("cb", [128, 512], F32, kind="ExternalInput").ap()
    y_d = dt_("y", [nseq, S, D], F32, kind="ExternalOutput").ap()
    xo_d = dt_("xo", [nseq, S, D], F32, kind="ExternalOutput").ap() if want_xo else None

    BN_S = nc.vector.BN_STATS_DIM
    BN_A = nc.vector.BN_AGGR_DIM
    es = ExitStack()
    with es:
        sems = [es.enter_context(nc.semaphore(f"s{i}")) for i in range(64)]
        sb = lambda name, shape, dt: es.enter_context(nc.sbuf_tensor("sb_" + name, shape, dt))
        xT = sb("xT", [128, 8, S], F32)
        hT = sb("hT", [128, 8, S], BF16)
        yatt = sb("yatt", [128, 4, S], BF16)
        yret = sb("yret", [128, 8, S], BF16)
        wring = sb("wring", [128, NS, 8, 128], BF16)
        cf = sb("cf", [128, CF_W], F32)
        cb = sb("cb", [128, 512], BF16)
        nwT = sb("nwT", [128, depth, 8], F32)
        gwT = sb("gwT", [128, depth, 8], F32)
        baT = sb("baT", [128, depth, 24], F32)
        fnT = sb("fnT", [128, 8], F32)
        cA = sb("cA", [128, 8, nseq], F32)
        modT = sb("modT", [128, depth, 24, nseq], F32)
        aT = sb("aT", [128, depth, 8, nseq], F32)
        AR = 9472
        arena = sb("arena", [128, AR], F32)
        psf = [es.enter_context(nc.psum_tensor(f"ps{i}", [128, 512], F32)) for i in range(7)]
        psb = es.enter_context(nc.psum_tensor("psb", [128, 1024], BF16))
        psbf = psb[:, :].bitcast(F32)

        identf = cf[:, 0:128]
        onesf = cf[:, 128:256]
        amask = cf[:, 256:768].rearrange("p (h n) -> p h n", h=4)
        qdec = cf[:, 768:772]
        kdec = cf[:, 772:776]
        theta = cf[:, 776:777]
        epsc = cf[:, 777:778]
        qdec2 = cf[:, 778:782]
        identb = cb[:, 0:128]
        onesb = cb[:, 128:256]
        maskb = cb[:, 256:512]

        P = Prog(nc, sems)
        B = Buf
        b_xT, b_hT, b_yatt, b_yret = B(), B(), B(), B()
        b_const, b_cb, b_small = B(), B(), B()
        b_ps = [B() for _ in range(7)]
        b_psb = B()
        b_xst = [B(), B()]
        ch_xst = [P.chan(), P.chan()]
        ch_cf, ch_cb, ch_posi = P.chan(), P.chan(), P.chan()
        ch_out = [P.chan(), P.chan()]
        b_ring = [B() for _ in range(NS)]
        ch_ring = [P.chan() for _ in range(NS)]
        ring_pos = [0]

        def carve(off, n, dt=F32):
            if dt == F32:
                assert off + n <= AR
                return arena[:, off:off + n]
            assert off + n // 2 <= AR
            return arena[:, off:off + n // 2].bitcast(BF16)

        def wload(src_ap, kc):
            slot = ring_pos[0] % NS
            ring_pos[0] += 1
            P.dma("pool", wring[:, slot, 0:kc, :], src_ap.rearrange("(c p) n -> p c n", p=128),
                  ch_ring[slot], writes=[b_ring[slot]])
            return slot

        def ring_align2():
            if ring_pos[0] % 2:
                ring_pos[0] += 1

        P.dma("sp", cf[:], cf_d[:, :], ch_cf, writes=[b_const])
        P.dma("pool", cb[:], cb_d[:, :], ch_cb, writes=[b_cb])
        b_p1, b_p2, b_p3, b_p4, b_p5 = B(), B(), B(), B(), B()
        ch_p = [P.chan() for _ in range(5)]
        for l in range(depth):
            P.dma("sp", nwT[:, l, :], nw_d[l].rearrange("(c p) -> p c", p=128), ch_p[0], writes=[b_p1], nonc=True)
            P.dma("sp", baT[:, l, :], bada_d[l].rearrange("(c p) -> p c", p=128), ch_p[1], writes=[b_p2], nonc=True)
            P.dma("sp", gwT[:, l, :], gnw_d[l].rearrange("(c p) -> p c", p=128), ch_p[4], writes=[b_p5], nonc=True)
        P.dma("sp", fnT[:], fnw_d.rearrange("(c p) -> p c", p=128), ch_p[2], writes=[b_p3], nonc=True)
        for bb in range(nseq):
            P.dma("sp", cA[:, :, bb], c_d[bb].rearrange("(c p) -> p c", p=128), ch_p[3], writes=[b_p4], nonc=True)
        P.op("act", ACT(cA[:], cA[:], AF.Silu), reads=[b_p4], writes=[b_p4])

        wst = [carve(i * 4096, 4096).rearrange("p (c n) -> p c n", c=8) for i in range(2)]
        b_wst = [B(), B()]
        ch_wst = [P.chan(), P.chan()]
        b_mod = B()
        it = 0
        for l in range(depth):
            for jg in range(6):
                k = it % 2
                it += 1
                P.dma("sp", wst[k], wada_d[l, :, jg * 512:(jg + 1) * 512].rearrange("(c p) n -> p c n", p=128),
                      ch_wst[k], writes=[b_wst[k]])
                mm = []
                for j4 in range(4):
                    for kc in range(8):
                        mm.append(MM(psf[0][:, j4 * nseq:(j4 + 1) * nseq], wst[k][:, kc, j4 * 128:(j4 + 1) * 128],
                                     cA[:, kc, :], start=(kc == 0), stop=(kc == 7)))
                P.op("pe", mm, reads=[b_wst[k], b_p4], writes=[b_ps[0]])
                P.op("dve", TT(modT[:, l, jg * 4:(jg + 1) * 4, :],
                               psf[0][:, 0:4 * nseq].rearrange("p (j b) -> p j b", j=4),
                               baT[:, l, jg * 4:(jg + 1) * 4].unsqueeze(2).broadcast_to([128, 4, nseq]), ALU.add),
                     reads=[b_ps[0], b_p2], writes=[b_mod])
        for l in range(depth):
            P.op("dve", STT(aT[:, l, :, :], modT[:, l, 8:16, :], 1.0,
                            nwT[:, l, :].unsqueeze(2).broadcast_to([128, 8, nseq]), ALU.add, ALU.mult),
                 reads=[b_mod, b_p1], writes=[b_small])

        def stats_rstd(t, rt, b_rt, sq, b_sq):
            ts = slice(t * 512, (t + 1) * 512)
            for c in range(8):
                k = c % 2
                P.op("act", ACT(sq[k], xT[:, c, ts], AF.Square), reads=[b_xT], writes=[b_sq[k]])
                P.op("pe", MM(psf[6][:, :], onesf, sq[k], start=(c == 0), stop=(c == 7)),
                     reads=[b_sq[k], b_const], writes=[b_ps[6]])
            P.op("act", ACT(rt, psf[6][:, :], AF.Sqrt, scale=1.0 / D, bias=epsc), reads=[b_ps[6], b_const], writes=[b_rt])
            P.op("dve", RCP(rt, rt), reads=[b_rt], writes=[b_rt])

        def proj_feat(slot, t, bank, kc=8, src=None, bs=None):
            src = hT if src is None else src
            bs = b_hT if bs is None else bs
            mm = [MM(psf[bank][:, :], wring[:, slot, c, :], src[:, c, t * 512:(t + 1) * 512],
                     start=(c == 0), stop=(c == kc - 1)) for c in range(kc)]
            P.op("pe", mm, reads=[b_ring[slot], bs], writes=[b_ps[bank]])

        evac_flip = [0]

        def evac_copy(out, in_, reads, writes, scale=None):
            evac_flip[0] ^= 1
            if evac_flip[0]:
                P.op("act", ACT(out, in_, AF.Copy, scale=scale), reads=reads, writes=writes)
            elif scale is None:
                P.op("dve", CP(out, in_), reads=reads, writes=writes)
            else:
                P.op("dve", TS(out, in_, scale, None, ALU.mult), reads=reads, writes=writes)

        for b in range(nseq):
            P.barrier()
            xst = [carve(0, 1024), carve(1024, 1024)]
            for tt in range(16):
                k = tt % 2
                P.dma("sp", xst[k], x_d[b, tt * 128:(tt + 1) * 128, :], ch_xst[k], writes=[b_xst[k]])
                for half in range(2):
                    bank = half
                    tr = [TR(psf[bank][:, q * 128:(q + 1) * 128], xst[k][:, (half * 4 + q) * 128:(half * 4 + q + 1) * 128], identf)
                          for q in range(4)]
                    P.op("pe", tr, reads=[b_xst[k], b_const], writes=[b_ps[bank]])
                    evac_copy(xT[:, half * 4:half * 4 + 4, tt * 128:(tt + 1) * 128],
                              psf[bank][:, :].rearrange("p (q n) -> p q n", q=4), [b_ps[bank]], [b_xT])

            for l in range(depth):
                P.barrier()
                sq = [carve(0, 512), carve(512, 512)]
                b_sq = [B(), B()]
                rt = carve(1024, 512)
                b_rt = B()
                tmp = [carve(1536, 512), carve(2048, 512)]
                b_tmp = [B(), B()]
                for t in range(4):
                    ts = slice(t * 512, (t + 1) * 512)
                    stats_rstd(t, rt, b_rt, sq, b_sq)
                    for c in range(8):
                        k = c % 2
                        P.op("dve", TT(tmp[k], xT[:, c, ts], rt, ALU.mult), reads=[b_xT, b_rt], writes=[b_tmp[k]])
                        P.op("act", ACT(hT[:, c, ts], tmp[k], AF.Identity, scale=aT[:, l, c, b:b + 1], bias=modT[:, l, c, b:b + 1]),
                             reads=[b_tmp[k], b_small, b_mod], writes=[b_hT])

                P.barrier()
                sza = carve(0, S, BF16)
                Uacc = carve(1024, S)
                Lacc = carve(3072, S)
                pT = [carve(5120, 512, BF16), carve(5376, 512, BF16)]
                rl = carve(5632, 512)
                qkv = []
                for i in range(2):
                    qkv.append(dict(q=yret[:, 4 * i, :], k=yret[:, 4 * i + 1, :], vT=yret[:, 4 * i + 2, :],
                                    v=yret[:, 4 * i + 3, :].rearrange("p (k n) -> p k n", k=16),
                                    bq=B(), bk=B(), bvT=B(), bv=B()))
                b_pT = [B(), B()]
                b_sza, b_U, b_L, b_rl = B(), B(), B(), B()
                gi = 0
                sc_i = 0
                DILS = (1, 4, 16)

                def ZA(s):
                    slot = wload(win_d[l, :, 4608 + s * 128: 4608 + (s + 1) * 128], 8)
                    for t in range(4):
                        bank = t % 2
                        proj_feat(slot, t, bank)
                        P.op("act", ACT(sza[:, t * 512:(t + 1) * 512], psf[bank][:, :], AF.Silu),
                             reads=[b_ps[bank]], writes=[b_sza])

                def PROJ(s, g, Q):
                    dil = DILS[g]
                    for name, off, bb, scale in (("q", 0, Q["bq"], 128 ** -0.5), ("k", 1536, Q["bk"], None),
                                                 ("vT", 3072, Q["bvT"], None)):
                        c0 = off + g * 512 + s * 128
                        slot = wload(win_d[l, :, c0:c0 + 128], 8)
                        dst = Q[name]
                        for t in range(4):
                            bank = t % 2
                            proj_feat(slot, t, bank)
                            m0 = t * 512 // dil
                            mw = 512 // dil
                            if dil == 1:
                                o_ap = dst[:, t * 512:(t + 1) * 512]
                                i_ap = psf[bank][:, :]
                            else:
                                o_ap = dst.rearrange("p (r m) -> p r m", r=dil)[:, :, m0:m0 + mw]
                                i_ap = psf[bank][:, :].rearrange("p (m r) -> p r m", r=dil)
                            evac_copy(o_ap, i_ap, [b_ps[bank]], [bb], scale=scale)

                def CORE(s, g, Q):
                    dil = DILS[g]
                    L = S // dil
                    CL = L // 128
                    for q4 in range(4):
                        tr = [TR(psb[:, q * 128:(q + 1) * 128], Q["vT"][:, (q4 * 4 + q) * 128:(q4 * 4 + q + 1) * 128], identb)
                              for q in range(4)]
                        P.op("pe", tr, reads=[Q["bvT"], b_cb], writes=[b_psb])
                        evac_copy(Q["v"][:, q4 * 4:q4 * 4 + 4, :], psb[:, 0:512].rearrange("p (q n) -> p q n", q=4),
                                  [b_psb], [Q["bv"]])
                    steps = []
                    for Bk in range(4):
                        q_lo = 4 * Bk
                        kb_lo = q_lo - 1 if (q_lo % CL) != 0 else q_lo
                        for kb in range(kb_lo, q_lo + 4):
                            qbs = []
                            if kb >= q_lo:
                                qbs.append(kb)
                            if (kb + 1) % CL != 0 and kb + 1 <= q_lo + 3:
                                qbs.append(kb + 1)
                            steps.append(dict(Bk=Bk, kb=kb, n=128 * len(qbs), q0=qbs[0], m_lo=(0 if qbs[0] == kb else 128),
                                              first=(kb == kb_lo), last=(kb == q_lo + 3), col=(qbs[0] - q_lo) * 128))

                    def emit_sc(st, i, Q=Q):
                        sb_i = i % 2
                        bankS = 2 + sb_i
                        n, kb, q0, m_lo = st["n"], st["kb"], st["q0"], st["m_lo"]
                        P.op("pe", [MM(psf[bankS][:, 0:n], Q["k"][:, kb * 128:(kb + 1) * 128],
                                       Q["q"][:, q0 * 128:q0 * 128 + n], start=True, stop=False),
                                    MM(psf[bankS][:, 0:n], identb, maskb[:, m_lo:m_lo + n], start=False, stop=True)],
                             reads=[Q["bq"], Q["bk"], b_cb], writes=[b_ps[bankS]])
                        P.op("act", ACT(pT[sb_i][:, 0:n], psf[bankS][:, 0:n], AF.Exp),
                             reads=[b_ps[bankS]], writes=[b_pT[sb_i]])

                    def emit_pv(st, i, Q=Q, dil=dil, g=g):
                        sb_i = i % 2
                        n, kb, col, first, last, Bk = st["n"], st["kb"], st["col"], st["first"], st["last"], st["Bk"]
                        if Bk % 2 == 0:
                            pO, bO, pL, bL = psf[4], b_ps[4], psf[5], b_ps[5]
                        else:
                            pO, bO, pL, bL = psf[6], b_ps[6], psbf, b_psb
                        P.op("pe", [MM(pO[:, col:col + n], Q["v"][:, kb, :], pT[sb_i][:, 0:n], start=first, stop=last, sg=True),
                                    MM(pL[:, col:col + n], onesb, pT[sb_i][:, 0:n], start=first, stop=last, sg=True)],
                             reads=[Q["bv"], b_pT[sb_i], b_cb], writes=[bO, bL])
                        if not last:
                            return
                        for acc, bacc, pbank, bbank in ((Uacc, b_U, pO, bO), (Lacc, b_L, pL, bL)):
                            if dil == 1:
                                o_ap = acc[:, Bk * 512:(Bk + 1) * 512]
                                i_ap = pbank[:, :]
                            elif dil == 4:
                                o_ap = acc.rearrange("p (m r) -> p r m", r=4)[:, Bk, :]
                                i_ap = pbank[:, :]
                            else:
                                o_ap = acc.rearrange("p (m r) -> p r m", r=16)[:, 4 * Bk:4 * Bk + 4, :]
                                i_ap = pbank[:, :].rearrange("p (r m) -> p r m", r=4)
                            if g == 0:
                                P.op("dve", CP(o_ap, i_ap), reads=[bbank], writes=[bacc])
                            else:
                                P.op("dve", TT(o_ap, i_ap, o_ap, ALU.add), reads=[bbank, bacc], writes=[bacc])

                    for i, st in enumerate(steps):
                        emit_sc(st, i)
                        if i >= 1:
                            emit_pv(steps[i - 1], i - 1)
                    emit_pv(steps[-1], len(steps) - 1)

                def COMBINE(s):
                    for t in range(4):
                        ts = slice(t * 512, (t + 1) * 512)
                        P.op("dve", RCP(rl, Lacc[:, ts]), reads=[b_L], writes=[b_rl])
                        P.op("dve", TT(rl, Uacc[:, ts], rl, ALU.mult), reads=[b_U, b_rl], writes=[b_rl])
                        P.op("dve", TT(yatt[:, s, ts], rl, sza[:, ts], ALU.mult), reads=[b_rl, b_sza], writes=[b_yatt])

                units = [(s, g) for s in range(4) for g in range(3)]
                PROJ(0, 0, qkv[0])
                for u, (s, g) in enumerate(units):
                    if u + 1 < len(units):
                        PROJ(units[u + 1][0], units[u + 1][1], qkv[(u + 1) % 2])
                    if g == 0:
                        ZA(s)
                    CORE(s, g, qkv[u % 2])
                    if g == 2:
                        COMBINE(s)

                P.barrier()
                HS = 512
                NCH = HS // 128
                cosT = carve(0, S)
                sinT = carve(2048, S)
                qT = carve(4096, 2 * HS, BF16).rearrange("p (e n) -> p e n", e=2)
                kT = carve(4608, 2 * HS, BF16).rearrange("p (e n) -> p e n", e=2)
                kd = carve(5120, NCH * 256, BF16).rearrange("p (c n) -> p c n", c=NCH)
                vr = carve(5632, NCH * 256, BF16).rearrange("p (c n) -> p c n", c=NCH)
                szr = carve(6144, NCH * 256, BF16).rearrange("p (c n) -> p c n", c=NCH)
                t1 = [carve(6656, 512), carve(7168, 512)]
                stf = carve(7680, 512)
                stb = [carve(8192, 512, BF16).rearrange("p (e n) -> p e n", e=2),
                       carve(8448, 512, BF16).rearrange("p (e n) -> p e n", e=2)]
                attS4 = carve(8704, NCH * 128, BF16).rearrange("p (c n) -> p c n", c=NCH)
                yr2 = carve(8960, 256)
                yr = carve(9216, 256, BF16)
                st6 = [carve(9344 + 16 * i, 8) for i in range(NCH)]
                mv = [carve(9344 + 16 * i + 8, 4) for i in range(NCH)]
                rs = [carve(9344 + 16 * i + 12, 1) for i in range(NCH)]
                nmr = [carve(9344 + 16 * i + 14, 1) for i in range(NCH)]
                b_cs = B()
                b_qT, b_kT, b_kd, b_vr, b_szr = B(), B(), B(), B(), B()
                b_t1 = [B(), B()]
                b_stf = B()
                b_stb = [B(), B()]
                b_attS4 = B()
                b_yr2, b_yr = B(), B()
                b_st = [B() for _ in range(NCH)]
                posi = stf.bitcast(I32)
                ang, ang2 = t1[0], t1[1]
                for t in range(4):
                    ts = slice(t * 512, (t + 1) * 512)
                    P.dma("sp", posi, pos_d[b:b + 1, ts].broadcast_to([128, 512]), ch_posi, writes=[b_stf])
                    P.op("dve", CP(ang, posi), reads=[b_stf], writes=[b_t1[0]])
                    P.op("dve", TS(ang, ang, theta, None, ALU.mult), reads=[b_t1[0], b_const], writes=[b_t1[0]])
                    MAGIC = 12582912.0
                    for dst, use_shift in ((sinT, False), (cosT, True)):
                        srcang = ang
                        bsrc = b_t1[0]
                        if use_shift:
                            P.op("dve", TS(stf, ang, PI / 2, None, ALU.add), reads=[b_t1[0]], writes=[b_stf])
                            srcang = stf
                            bsrc = b_stf
                        P.op("dve", TS(ang2, srcang, 1.0 / (2 * PI), MAGIC, ALU.mult, ALU.add), reads=[bsrc], writes=[b_t1[1]])
                        P.op("dve", TS(ang2, ang2, -MAGIC, None, ALU.add), reads=[b_t1[1]], writes=[b_t1[1]])
                        P.op("dve", STT(ang2, ang2, -2 * PI, srcang, ALU.mult, ALU.add), reads=[b_t1[1], bsrc], writes=[b_t1[1]])
                        P.op("act", ACT(dst[:, ts], ang2, AF.Sin, scale=0.999999), reads=[b_t1[1]], writes=[b_cs])
                rot_i = 0
                for h in range(4):
                    ring_align2()
                    wcol = lambda base, e: win_d[l, :, base + h * 256 + e * 128: base + h * 256 + (e + 1) * 128]
                    s_q = [wload(wcol(5120, e), 8) for e in range(2)]
                    s_k = [wload(wcol(6144, e), 8) for e in range(2)]
                    s_v = [wload(wcol(7168, e), 8) for e in range(2)]
                    s_z = [wload(wcol(8192, e), 8) for e in range(2)]
                    assert s_v[1] == s_v[0] + 1 and s_z[1] == s_z[0] + 1
                    for hf in range(S // HS):
                        for slots, dstT, bdst in ((s_q, qT, b_qT), (s_k, kT, b_kT)):
                            for tl in range(HS // 512):
                                t = hf * (HS // 512) + tl
                                ts = slice(t * 512, (t + 1) * 512)
                                tls = slice(tl * 512, (tl + 1) * 512)
                                pb = (rot_i % 2) * 2
                                rot_i += 1
                                proj_feat(slots[0], t, pb)
                                proj_feat(slots[1], t, pb + 1)
                                pA, pB = psf[pb][:, :], psf[pb + 1][:, :]
                                rd = [b_ps[pb], b_ps[pb + 1], b_cs]
                                P.op("dve", TT(t1[0], pA, cosT[:, ts], ALU.mult), reads=rd, writes=[b_t1[0]])
                                P.op("dve", TT(t1[1], pB, sinT[:, ts], ALU.mult), reads=rd, writes=[b_t1[1]])
                                P.op("dve", TT(dstT[:, 0, tls], t1[0], t1[1], ALU.subtract), reads=b_t1, writes=[bdst])
                                P.op("dve", TT(t1[0], pB, cosT[:, ts], ALU.mult), reads=rd, writes=[b_t1[0]])
                                P.op("dve", TT(t1[1], pA, sinT[:, ts], ALU.mult), reads=rd, writes=[b_t1[1]])
                                P.op("dve", TT(dstT[:, 1, tls], t1[0], t1[1], ALU.add), reads=b_t1, writes=[bdst])
                        def TOKPROJ(slots, dst, bdst, fn, hf=hf):
                            for c2 in range(NCH // 2):
                                bank = 4 + (c2 % 2)
                                mm = []
                                for q in range(2):
                                    tok0 = hf * HS + (c2 * 2 + q) * 128
                                    for c in range(8):
                                        mm.append(MM(psf[bank][:, q * 256:(q + 1) * 256], hT[:, c, tok0:tok0 + 128],
                                                     wring[:, slots[0]:slots[0] + 2, c, :], start=(c == 0), stop=(c == 7), sg=True))
                                P.op("pe", mm, reads=[b_hT, b_ring[slots[0]], b_ring[slots[1]]], writes=[b_ps[bank]])
                                o_ap = dst[:, c2 * 2:c2 * 2 + 2, :]
                                i_ap = psf[bank][:, :].rearrange("p (q n) -> p q n", q=2)
                                if fn is None:
                                    evac_copy(o_ap, i_ap, [b_ps[bank]], [bdst])
                                else:
                                    P.op("act", ACT(o_ap, i_ap, AF.Silu), reads=[b_ps[bank]], writes=[bdst])
                        TOKPROJ(s_v, vr, b_vr, None)
                        for c2 in range(NCH // 2):
                            tr = []
                            for q in range(2):
                                cc = c2 * 2 + q
                                for e2 in range(2):
                                    tr.append(TR(psb[:, (q * 2 + e2) * 128:(q * 2 + e2 + 1) * 128], kT[:, e2, cc * 128:(cc + 1) * 128], identb))
                            P.op("pe", tr, reads=[b_kT, b_cb], writes=[b_psb])
                            P.op("act", ACT(kd[:, c2 * 2:c2 * 2 + 2, :], psb[:, 0:512].rearrange("p (q n) -> p q n", q=2),
                                            AF.Copy, scale=kdec[:, h:h + 1]), reads=[b_psb, b_const], writes=[b_kd])
                        gcs = [hf * NCH + cc for cc in range(NCH)]
                        att_mm = []
                        for cc in range(NCH):
                            cs = slice(cc * 128, (cc + 1) * 128)
                            att_mm.append(MM(psf[6][:, cc * 128:(cc + 1) * 128], kT[:, 0, cs], qT[:, 0, cs], start=True, stop=False, sg=True))
                            att_mm.append(MM(psf[6][:, cc * 128:(cc + 1) * 128], kT[:, 1, cs], qT[:, 1, cs], start=False, stop=True, sg=True))
                        P.op("pe", att_mm, reads=[b_kT, b_qT], writes=[b_ps[6]])
                        P.op("dve", TT(attS4, psf[6][:, :].rearrange("p (c n) -> p c n", c=NCH),
                                       amask[:, h, :].unsqueeze(1).broadcast_to([128, NCH, 128]), ALU.mult),
                             reads=[b_ps[6], b_const], writes=[b_attS4])

                        def emit_kv(cc):
                            gc = gcs[cc]
                            if gc >= 15:
                                return
                            bank = 4 + (cc % 2)
                            P.op("pe", [MM(psf[bank][:, 0:256], kd[:, cc, 0:128], vr[:, cc, :], sg=True),
                                        MM(psf[bank][:, 256:512], kd[:, cc, 128:256], vr[:, cc, :], sg=True)],
                                 reads=[b_kd, b_vr], writes=[b_ps[bank]])
                            if gc == 0:
                                P.op("dve", CP(stf, psf[bank][:, :]), reads=[b_ps[bank]], writes=[b_stf])
                            else:
                                P.op("dve", STT(stf, stf, CDEC[h], psf[bank][:, :], ALU.mult, ALU.add),
                                     reads=[b_ps[bank], b_stf], writes=[b_stf])
                            P.op("act", ACT(stb[gc % 2], stf.rearrange("p (e n) -> p e n", e=2), AF.Copy),
                                 reads=[b_stf], writes=[b_stb[gc % 2]])

                        def emit_G(cc):
                            gc = gcs[cc]
                            cs = slice(cc * 128, (cc + 1) * 128)
                            mm = [MM(psf[cc][:, 0:256], attS4[:, cc, :], vr[:, cc, :], start=True, stop=(gc == 0))]
                            rd = [b_attS4, b_vr]
                            if gc > 0:
                                sbi = (gc - 1) % 2
                                mm.append(MM(psf[cc][:, 0:256], qT[:, 0, cs], stb[sbi][:, 0, :], start=False, stop=False))
                                mm.append(MM(psf[cc][:, 0:256], qT[:, 1, cs], stb[sbi][:, 1, :], start=False, stop=True))
                                rd += [b_qT, b_stb[sbi]]
                            P.op("pe", mm, reads=rd, writes=[b_ps[cc]])
                            P.op("dve", ("bn_stats", dict(out=st6[cc][:, 0:BN_S], in_=psf[cc][:, 0:256])), reads=[b_ps[cc]], writes=[b_st[cc]])
                            P.op("dve", ("bn_aggr", dict(out=mv[cc][:, 0:BN_A], in_=st6[cc][:, 0:BN_S])), reads=[b_st[cc]], writes=[b_st[cc]])
                            if USE_POW:
                                P.op("dve", TT(rs[cc], mv[cc][:, 1:2], qdec2[:, h:h + 1], ALU.mult), reads=[b_st[cc], b_const], writes=[b_st[cc]])
                                P.op("dve", TS(rs[cc], rs[cc], EPS, -0.5, ALU.add, ALU.pow), reads=[b_st[cc]], writes=[b_st[cc]])
                            else:
                                P.op("act", ACT(rs[cc], mv[cc][:, 1:2], AF.Sqrt, scale=qdec2[:, h:h + 1], bias=epsc),
                                     reads=[b_st[cc], b_const], writes=[b_st[cc]])
                                P.op("dve", RCP(rs[cc], rs[cc]), reads=[b_st[cc]], writes=[b_st[cc]])
                            P.op("dve", TT(rs[cc], rs[cc], qdec[:, h:h + 1], ALU.mult), reads=[b_st[cc], b_const], writes=[b_st[cc]])
                            P.op("dve", STT(nmr[cc], mv[cc][:, 0:1], -1.0, rs[cc], ALU.mult, ALU.mult), reads=[b_st[cc]], writes=[b_st[cc]])

                        def emit_tail(cc):
                            gc = gcs[cc]
                            P.op("act", ACT(yr2, psf[cc][:, 0:256], AF.Identity, scale=rs[cc], bias=nmr[cc]),
                                 reads=[b_ps[cc], b_st[cc]], writes=[b_yr2])
                            P.op("dve", TT(yr, yr2, szr[:, cc, :], ALU.mult), reads=[b_yr2, b_szr], writes=[b_yr])
                            P.op("pe", [TR(psb[:, 512:640], yr[:, 0:128], identb), TR(psb[:, 640:768], yr[:, 128:256], identb)],
                                 reads=[b_yr, b_cb], writes=[b_psb])
                            tok0 = gc * 128
                            for q in range(2):
                                P.op("act", ACT(yret[:, 2 * h + q, tok0:tok0 + 128], psb[:, 512 + q * 128:640 + q * 128],
                                                AF.Copy, scale=gwT[:, l, 2 * h + q:2 * h + q + 1]),
                                     reads=[b_psb, b_p5], writes=[b_yret])

                        emit_kv(0)
                        emit_G(0)
                        emit_kv(1)
                        emit_G(1)
                        TOKPROJ(s_z, szr, b_szr, AF.Silu)
                        emit_tail(0)
                        emit_kv(2)
                        emit_G(2)
                        emit_tail(1)
                        emit_kv(3)
                        emit_G(3)
                        emit_tail(2)
                        emit_tail(3)

                P.barrier()
                mg = carve(0, 8 * S, BF16).rearrange("p (c n) -> p c n", c=8)
                sg1 = carve(8192, 512, BF16)
                sg_ = [sg1, sg1]
                m1 = [carve(8448, 512), carve(8960, 512)]
                bsg1 = B()
                b_mg, b_sg, b_m1 = B(), [bsg1, bsg1], [B(), B()]
                for j in range(8):
                    s_ga = wload(win_d[l, :, 9216 + j * 128: 9216 + (j + 1) * 128], 8)
                    s_gr = wload(win_d[l, :, 10240 + j * 128: 10240 + (j + 1) * 128], 8)
                    s_pa = wload(wpa_d[l, :, j * 128:(j + 1) * 128], 4)
                    s_pr = wload(wpr_d[l, :, j * 128:(j + 1) * 128], 8)
                    for t in range(4):
                        ts = slice(t * 512, (t + 1) * 512)
                        proj_feat(s_ga, t, 0)
                        proj_feat(s_gr, t, 1)
                        proj_feat(s_pa, t, 2, kc=4, src=yatt, bs=b_yatt)
                        proj_feat(s_pr, t, 3, kc=8, src=yret, bs=b_yret)
                        for i in range(2):
                            P.op("act", ACT(sg_[i], psf[i][:, :], AF.Sigmoid), reads=[b_ps[i]], writes=[b_sg[i]])
                            P.op("dve", TT(m1[i], psf[2 + i][:, :], sg_[i], ALU.mult), reads=[b_ps[2 + i], b_sg[i]], writes=[b_m1[i]])
                        P.op("dve", TT(mg[:, j, ts], m1[0], m1[1], ALU.add), reads=b_m1, writes=[b_mg])
                for j2 in range(8):
                    s_o = wload(wout_d[l, :, j2 * 128:(j2 + 1) * 128], 8)
                    for t in range(4):
                        ts = slice(t * 512, (t + 1) * 512)
                        bank = 4 + (t % 2)
                        proj_feat(s_o, t, bank, kc=8, src=mg, bs=b_mg)
                        P.op("dve", STT(xT[:, j2, ts], psf[bank][:, :], modT[:, l, 16 + j2, b:b + 1], xT[:, j2, ts], ALU.mult, ALU.add),
                             reads=[b_ps[bank], b_xT, b_mod], writes=[b_xT])

            P.barrier()
            sq = [carve(0, 512), carve(512, 512)]
            b_sq = [B(), B()]
            rt = carve(1024, 512)
            b_rt = B()
            tmp1 = carve(1536, 512)
            b_tmp1 = B()
            yT = carve(2048, 4096).rearrange("p (c n) -> p c n", c=8)
            b_yT = B()
            ost = [carve(6144, 1024), carve(7168, 1024)]
            b_ost = [B(), B()]
            oi = 0
            outs = [(True, y_d)] + ([(False, xo_d)] if want_xo else [])
            for t in range(4):
                ts = slice(t * 512, (t + 1) * 512)
                stats_rstd(t, rt, b_rt, sq, b_sq)
                for normed, dst_d in outs:
                    if normed:
                        for c in range(8):
                            P.op("dve", TT(tmp1, xT[:, c, ts], rt, ALU.mult), reads=[b_xT, b_rt], writes=[b_tmp1])
                            P.op("act", ACT(yT[:, c, :], tmp1, AF.Copy, scale=fnT[:, c:c + 1]), reads=[b_tmp1, b_p3], writes=[b_yT])
                    for tb in range(4):
                        k = oi % 2
                        oi += 1
                        for half in range(2):
                            bank = half
                            tr = []
                            for q in range(4):
                                c = half * 4 + q
                                src = yT[:, c, tb * 128:(tb + 1) * 128] if normed else xT[:, c, t * 512 + tb * 128: t * 512 + (tb + 1) * 128]
                                tr.append(TR(psf[bank][:, q * 128:(q + 1) * 128], src, identf))
                            P.op("pe", tr, reads=[b_yT if normed else b_xT, b_const], writes=[b_ps[bank]])
                            evac_copy(ost[k][:, half * 512:(half + 1) * 512], psf[bank][:, :], [b_ps[bank]], [b_ost[k]])
                        tok0 = t * 512 + tb * 128
                        P.dma("sp", dst_d[b, tok0:tok0 + 128, :], ost[k], ch_out[k], reads=[b_ost[k]])
        P.final_wait("sp", ch_out)
        with nc.Block() as block:
            P.replay(block)
    return nc


_CACHE = {}


def _get_prog(depth, nseq, want_xo):
    key = (depth, nseq, want_xo)
    if key not in _CACHE:
        _CACHE[key] = build(depth, nseq, want_xo)
    return _CACHE[key]


MODE = "fused"


def kernel(x, c, positions, norm_w, w_ada, b_ada, w_in, ret_gn_w, w_proj_attn, w_proj_ret, w_out, final_norm_w):
    x = np.asarray(x, dtype=np.float32)
    Bt = x.shape[0]
    depth = int(np.asarray(norm_w).shape[0])
    cf, cb, _ = host_consts()
    f = lambda a: np.ascontiguousarray(np.asarray(a, dtype=np.float32))
    c = f(c)
    positions = np.ascontiguousarray(np.asarray(positions, dtype=np.int32))
    norm_w, w_ada, b_ada, w_in, ret_gn_w = f(norm_w), f(w_ada), f(b_ada), f(w_in), f(ret_gn_w)
    w_proj_attn, w_proj_ret, w_out, final_norm_w = f(w_proj_attn), f(w_proj_ret), f(w_out), f(final_norm_w)

    def launch(xin, rows, nseq, lsl, want_xo):
        d = lsl.stop - lsl.start
        nc = _get_prog(d, nseq, want_xo)
        in_maps = []
        for i in range(NCORES):
            in_maps.append({
                "x": np.ascontiguousarray(xin[i]), "c": np.ascontiguousarray(c[rows[i]]),
                "pos": np.ascontiguousarray(positions[rows[i]]),
                "norm_w": norm_w[lsl], "w_ada": w_ada[lsl], "b_ada": b_ada[lsl], "w_in": w_in[lsl], "gn_w": ret_gn_w[lsl],
                "w_pa": w_proj_attn[lsl], "w_pr": w_proj_ret[lsl], "w_out": w_out[lsl], "fnw": final_norm_w,
                "cf": cf, "cb": cb})
        res = run_bass_kernel_spmd(nc, in_maps, core_ids=list(range(NCORES)))
        y = np.stack([r["y"] for r in res.results], axis=0)
        xo = np.stack([r["xo"] for r in res.results], axis=0) if want_xo else None
        return y, xo

    out = np.empty_like(x)
    if MODE == "fused":
        nseq = Bt // NCORES
        rows = np.arange(Bt).reshape(NCORES, nseq)
        y, _ = launch(x[rows], rows, nseq, slice(0, depth), False)
        out[rows] = y
        return out
    ngrp = Bt // NCORES
    for g in range(ngrp):
        rows = (g * NCORES + np.arange(NCORES)).reshape(NCORES, 1)
        if MODE == "layers4":
            y, _ = launch(x[rows], rows, 1, slice(0, depth), False)
        else:
            cur = x[rows]
            for l in range(depth):
                y, cur = launch(cur, rows, 1, slice(l, l + 1), True)
        out[rows] = y
    return out
```

```python
import math
import numpy as np
from contextlib import ExitStack
import concourse.bass as bass
import concourse.mybir as mybir
from concourse.bass_utils import run_bass_kernel_spmd

F32 = mybir.dt.float32
BF16 = mybir.dt.bfloat16
I32 = mybir.dt.int32
AF = mybir.ActivationFunctionType
ALU = mybir.AluOpType

S = 2048
D = 1024
INW = 11264
EPS = 1e-6
NCORES = 8
USE_POW = False
NS = 10
PI = math.pi


class Buf:
    __slots__ = ("w", "r")

    def __init__(self):
        self.w = {}
        self.r = {}


class Eng:
    def __init__(self, name, sem):
        self.name = name
        self.sem = sem
        self.count = 0
        self.waited = {}
        self.items = []


class Chan:
    def __init__(self, sem):
        self.sem = sem
        self.count = 0


class Prog:
    def __init__(self, nc, sems):
        self.nc = nc
        self.free_sems = list(sems)
        self.E = {n: Eng(n, self.free_sems.pop()) for n in ("pe", "act", "dve", "pool", "sp")}
        self.chans = []

    def chan(self):
        c = Chan(self.free_sems.pop())
        self.chans.append(c)
        return c

    def _deps(self, reads, writes):
        deps = {}
        for b in reads:
            for s, v in b.w.items():
                if deps.get(s, 0) < v:
                    deps[s] = v
        for b in writes:
            for s, v in b.w.items():
                if deps.get(s, 0) < v:
                    deps[s] = v
            for s, v in b.r.items():
                if deps.get(s, 0) < v:
                    deps[s] = v
        return deps

    def _emit_waits(self, e, deps, skip_own=False):
        for s, v in deps.items():
            if skip_own and s is e.sem:
                continue
            if e.waited.get(s, 0) < v:
                e.items.append(("w", s, v))
                e.waited[s] = v

    def _commit(self, tok, reads, writes):
        s, v = tok
        for b in reads:
            if b.r.get(s, 0) < v:
                b.r[s] = v
        for b in writes:
            b.w = {s: v}
            b.r = {}

    def op(self, eng, insts, reads=(), writes=()):
        if isinstance(insts, tuple):
            insts = [insts]
        e = self.E[eng]
        self._emit_waits(e, self._deps(reads, writes), skip_own=(eng == "pe"))
        e.count += 1
        e.items.append(("o", insts, e.sem, 1))
        self._commit((e.sem, e.count), reads, writes)

    def dma(self, eng, out, in_, chan, reads=(), writes=(), nonc=False):
        e = self.E[eng]
        self._emit_waits(e, self._deps(reads, writes))
        chan.count += 16
        e.items.append(("d" if nonc else "o", [("dma_start", dict(out=out, in_=in_))], chan.sem, 16))
        self._commit((chan.sem, chan.count), reads, writes)

    def barrier(self, engs=("pe", "act", "dve", "sp")):
        deps = {}
        for n in engs:
            e = self.E[n]
            if e.count and n != "sp":
                deps[e.sem] = e.count
        for n in engs:
            self._emit_waits(self.E[n], deps)

    def final_wait(self, eng, chans):
        e = self.E[eng]
        self._emit_waits(e, {c.sem: c.count for c in chans if c.count})

    def replay(self, block):
        nc = self.nc

        def run(e, engobj):
            for it in e.items:
                if it[0] == "w":
                    engobj.wait_ge(it[1], it[2])
                elif it[0] == "d":
                    with nc.allow_non_contiguous_dma(reason="tiny parameter layout loads"):
                        for name, kw in it[1]:
                            inst = getattr(engobj, name)(**kw)
                    inst.then_inc(it[2], it[3])
                else:
                    for name, kw in it[1]:
                        inst = getattr(engobj, name)(**kw)
                    inst.then_inc(it[2], it[3])

        @block.tensor
        def _(eng):
            run(self.E["pe"], eng)

        @block.scalar
        def _(eng):
            run(self.E["act"], eng)

        @block.vector
        def _(eng):
            run(self.E["dve"], eng)

        @block.gpsimd
        def _(eng):
            run(self.E["pool"], eng)

        @block.sync
        def _(eng):
            run(self.E["sp"], eng)


def host_consts():
    ident = np.eye(128, dtype=np.float32)
    ones = np.ones((128, 128), dtype=np.float32)
    j = np.arange(128)[:, None]
    i = np.arange(128)[None, :]
    mask = np.zeros((128, 256), dtype=np.float32)
    mask[:, 0:128] = np.where(i >= j, 0.0, -30000.0)
    mask[:, 128:256] = np.where(i <= j, 0.0, -30000.0)
    gam = 1.0 - np.exp2(-5.0 - np.arange(4, dtype=np.float64))
    lg = np.log(gam)
    m = np.arange(128)[:, None]
    n = np.arange(128)[None, :]
    amask = np.zeros((128, 4, 128), dtype=np.float32)
    qdec = np.zeros((128, 4), dtype=np.float32)
    kdec = np.zeros((128, 4), dtype=np.float32)
    for h in range(4):
        amask[:, h, :] = np.where(n >= m, np.exp(-lg[h] * (m + 1.0)), 0.0) / 16.0
        qdec[:, h] = np.exp(lg[h] * (np.arange(128) + 1.0))
        kdec[:, h] = np.exp(lg[h] * (127.0 - np.arange(128))) / 16.0
    cdec = [float(np.exp(lg[h] * 128.0)) for h in range(4)]
    theta = (np.float32(10000.0) ** (-(np.arange(128, dtype=np.float32) / np.float32(128.0)))).astype(np.float32)[:, None]
    cf = np.concatenate([ident, ones, amask.reshape(128, 512), qdec, kdec, theta,
                         np.full((128, 1), EPS, np.float32), qdec * qdec], axis=1)
    cb = np.concatenate([ident, ones, mask], axis=1)
    return np.ascontiguousarray(cf), np.ascontiguousarray(cb), cdec


CF_W = 128 + 128 + 512 + 4 + 4 + 1 + 1 + 4
_, _, CDEC = host_consts()


def MM(out, lhsT, rhs, start=True, stop=True, sg=False):
    kw = dict(out=out, lhsT=lhsT, rhs=rhs, start=start, stop=stop)
    if sg:
        kw["skip_group_check"] = True
    return ("matmul", kw)


def TR(out, in_, ident):
    return ("transpose", dict(out=out, in_=in_, identity=ident))


def ACT(out, in_, func, scale=None, bias=None):
    kw = dict(out=out, in_=in_, func=func)
    if scale is not None:
        kw["scale"] = scale
    if bias is not None:
        kw["bias"] = bias
    return ("activation", kw)


def TT(out, in0, in1, op):
    return ("tensor_tensor", dict(out=out, in0=in0, in1=in1, op=op))


def TS(out, in0, s1, s2, op0, op1=None):
    kw = dict(out=out, in0=in0, scalar1=s1, scalar2=s2, op0=op0)
    if op1 is not None:
        kw["op1"] = op1
    return ("tensor_scalar", kw)


def STT(out, in0, scalar, in1, op0, op1):
    return ("scalar_tensor_tensor", dict(out=out, in0=in0, scalar=scalar, in1=in1, op0=op0, op1=op1))


def CP(out, in_):
    return ("tensor_copy", dict(out=out, in_=in_))


def RCP(out, in_):
    return ("reciprocal", dict(out=out, in_=in_))


def build(depth, nseq, want_xo):
    nc = bass.Bass("TRN2", target_bir_lowering=False)
    dt_ = nc.dram_tensor
    x_d = dt_("x", [nseq, S, D], F32, kind="ExternalInput").ap()
    c_d = dt_("c", [nseq, D], F32, kind="ExternalInput").ap()
    pos_d = dt_("pos", [nseq, S], I32, kind="ExternalInput").ap()
    nw_d = dt_("norm_w", [depth, D], F32, kind="ExternalInput").ap()
    wada_d = dt_("w_ada", [depth, D, 3 * D], F32, kind="ExternalInput").ap()
    bada_d = dt_("b_ada", [depth, 3 * D], F32, kind="ExternalInput").ap()
    win_d = dt_("w_in", [depth, D, INW], F32, kind="ExternalInput").ap()
    gnw_d = dt_("gn_w", [depth, D], F32, kind="ExternalInput").ap()
    wpa_d = dt_("w_pa", [depth, 512, D], F32, kind="ExternalInput").ap()
    wpr_d = dt_("w_pr", [depth, D, D], F32, kind="ExternalInput").ap()
    wout_d = dt_("w_out", [depth, D, D], F32, kind="ExternalInput").ap()
    fnw_d = dt_("fnw", [D], F32, kind="ExternalInput").ap()
    cf_d = dt_("cf", [128, CF_W], F32, kind="ExternalInput").ap()
    cb_d = dt_("cb", [128, 512], F32, kind="ExternalInput").ap()
    y_d = dt_("y", [nseq, S, D], F32, kind="ExternalOutput").ap()
    xo_d = dt_("xo", [nseq, S, D], F32, kind="ExternalOutput").ap() if want_xo else None

    BN_S = nc.vector.BN_STATS_DIM
    BN_A = nc.vector.BN_AGGR_DIM
    es = ExitStack()
    with es:
        sems = [es.enter_context(nc.semaphore(f"s{i}")) for i in range(64)]
        sb = lambda name, shape, dt: es.enter_context(nc.sbuf_tensor("sb_" + name, shape, dt))
        xT = sb("xT", [128, 8, S], F32)
        hT = sb("hT", [128, 8, S], BF16)
        yatt = sb("yatt", [128, 4, S], BF16)
        yret = sb("yret", [128, 8, S], BF16)
        wring = sb("wring", [128, NS, 8, 128], BF16)
        cf = sb("cf", [128, CF_W], F32)
        cb = sb("cb", [128, 512], BF16)
        nwT = sb("nwT", [128, depth, 8], F32)
        gwT = sb("gwT", [128, depth, 8], F32)
        baT = sb("baT", [128, depth, 24], F32)
        fnT = sb("fnT", [128, 8], F32)
        cA = sb("cA", [128, 8, nseq], F32)
        modT = sb("modT", [128, depth, 24, nseq], F32)
        aT = sb("aT", [128, depth, 8, nseq], F32)
        AR = 9472
        arena = sb("arena", [128, AR], F32)
        psf = [es.enter_context(nc.psum_tensor(f"ps{i}", [128, 512], F32)) for i in range(7)]
        psb = es.enter_context(nc.psum_tensor("psb", [128, 1024], BF16))
        psbf = psb[:, :].bitcast(F32)

        identf = cf[:, 0:128]
        onesf = cf[:, 128:256]
        amask = cf[:, 256:768].rearrange("p (h n) -> p h n", h=4)
        qdec = cf[:, 768:772]
        kdec = cf[:, 772:776]
        theta = cf[:, 776:777]
        epsc = cf[:, 777:778]
        qdec2 = cf[:, 778:782]
        identb = cb[:, 0:128]
        onesb = cb[:, 128:256]
        maskb = cb[:, 256:512]

        P = Prog(nc, sems)
        B = Buf
        b_xT, b_hT, b_yatt, b_yret = B(), B(), B(), B()
        b_const, b_cb, b_small = B(), B(), B()
        b_ps = [B() for _ in range(7)]
        b_psb = B()
        b_xst = [B(), B()]
        ch_xst = [P.chan(), P.chan()]
        ch_cf, ch_cb, ch_posi = P.chan(), P.chan(), P.chan()
        ch_out = [P.chan(), P.chan()]
        b_ring = [B() for _ in range(NS)]
        ch_ring = [P.chan() for _ in range(NS)]
        ring_pos = [0]

        def carve(off, n, dt=F32):
            if dt == F32:
                assert off + n <= AR
                return arena[:, off:off + n]
            assert off + n // 2 <= AR
            return arena[:, off:off + n // 2].bitcast(BF16)

        def wload(src_ap, kc):
            slot = ring_pos[0] % NS
            ring_pos[0] += 1
            P.dma("pool", wring[:, slot, 0:kc, :], src_ap.rearrange("(c p) n -> p c n", p=128),
                  ch_ring[slot], writes=[b_ring[slot]])
            return slot

        def ring_align2():
            if ring_pos[0] % 2:
                ring_pos[0] += 1

        P.dma("sp", cf[:], cf_d[:, :], ch_cf, writes=[b_const])
        P.dma("pool", cb[:], cb_d[:, :], ch_cb, writes=[b_cb])
        b_p1, b_p2, b_p3, b_p4, b_p5 = B(), B(), B(), B(), B()
        ch_p = [P.chan() for _ in range(5)]
        for l in range(depth):
            P.dma("sp", nwT[:, l, :], nw_d[l].rearrange("(c p) -> p c", p=128), ch_p[0], writes=[b_p1], nonc=True)
            P.dma("sp", baT[:, l, :], bada_d[l].rearrange("(c p) -> p c", p=128), ch_p[1], writes=[b_p2], nonc=True)
            P.dma("sp", gwT[:, l, :], gnw_d[l].rearrange("(c p) -> p c", p=128), ch_p[4], writes=[b_p5], nonc=True)
        P.dma("sp", fnT[:], fnw_d.rearrange("(c p) -> p c", p=128), ch_p[2], writes=[b_p3], nonc=True)
        for bb in range(nseq):
            P.dma("sp", cA[:, :, bb], c_d[bb].rearrange("(c p) -> p c", p=128), ch_p[3], writes=[b_p4], nonc=True)
        P.op("act", ACT(cA[:], cA[:], AF.Silu), reads=[b_p4], writes=[b_p4])

        wst = [carve(i * 4096, 4096).rearrange("p (c n) -> p c n", c=8) for i in range(2)]
        b_wst = [B(), B()]
        ch_wst = [P.chan(), P.chan()]
        b_mod = B()
        it = 0
        for l in range(depth):
            for jg in range(6):
                k = it % 2
                it += 1
                P.dma("sp", wst[k], wada_d[l, :, jg * 512:(jg + 1) * 512].rearrange("(c p) n -> p c n", p=128),
                      ch_wst[k], writes=[b_wst[k]])
                mm = []
                for j4 in range(4):
                    for kc in range(8):
                        mm.append(MM(psf[0][:, j4 * nseq:(j4 + 1) * nseq], wst[k][:, kc, j4 * 128:(j4 + 1) * 128],
                                     cA[:, kc, :], start=(kc == 0), stop=(kc == 7)))
                P.op("pe", mm, reads=[b_wst[k], b_p4], writes=[b_ps[0]])
                P.op("dve", TT(modT[:, l, jg * 4:(jg + 1) * 4, :],
                               psf[0][:, 0:4 * nseq].rearrange("p (j b) -> p j b", j=4),
                               baT[:, l, jg * 4:(jg + 1) * 4].unsqueeze(2).broadcast_to([128, 4, nseq]), ALU.add),
                     reads=[b_ps[0], b_p2], writes=[b_mod])
        for l in range(depth):
            P.op("dve", STT(aT[:, l, :, :], modT[:, l, 8:16, :], 1.0,
                            nwT[:, l, :].unsqueeze(2).broadcast_to([128, 8, nseq]), ALU.add, ALU.mult),
                 reads=[b_mod, b_p1], writes=[b_small])

        def stats_rstd(t, rt, b_rt, sq, b_sq):
            ts = slice(t * 512, (t + 1) * 512)
            for c in range(8):
                k = c % 2
                P.op("act", ACT(sq[k], xT[:, c, ts], AF.Square), reads=[b_xT], writes=[b_sq[k]])
                P.op("pe", MM(psf[6][:, :], onesf, sq[k], start=(c == 0), stop=(c == 7)),
                     reads=[b_sq[k], b_const], writes=[b_ps[6]])
            P.op("act", ACT(rt, psf[6][:, :], AF.Sqrt, scale=1.0 / D, bias=epsc), reads=[b_ps[6], b_const], writes=[b_rt])
            P.op("dve", RCP(rt, rt), reads=[b_rt], writes=[b_rt])

        def proj_feat(slot, t, bank, kc=8, src=None, bs=None):
            src = hT if src is None else src
            bs = b_hT if bs is None else bs
            mm = [MM(psf[bank][:, :], wring[:, slot, c, :], src[:, c, t * 512:(t + 1) * 512],
                     start=(c == 0), stop=(c == kc - 1)) for c in range(kc)]
            P.op("pe", mm, reads=[b_ring[slot], bs], writes=[b_ps[bank]])

        evac_flip = [0]

        def evac_copy(out, in_, reads, writes, scale=None):
            evac_flip[0] ^= 1
            if evac_flip[0]:
                P.op("act", ACT(out, in_, AF.Copy, scale=scale), reads=reads, writes=writes)
            elif scale is None:
                P.op("dve", CP(out, in_), reads=reads, writes=writes)
            else:
                P.op("dve", TS(out, in_, scale, None, ALU.mult), reads=reads, writes=writes)

        for b in range(nseq):
            P.barrier()
            xst = [carve(0, 1024), carve(1024, 1024)]
            for tt in range(16):
                k = tt % 2
                P.dma("sp", xst[k], x_d[b, tt * 128:(tt + 1) * 128, :], ch_xst[k], writes=[b_xst[k]])
                for half in range(2):
                    bank = half
                    tr = [TR(psf[bank][:, q * 128:(q + 1) * 128], xst[k][:, (half * 4 + q) * 128:(half * 4 + q + 1) * 128], identf)
                          for q in range(4)]
                    P.op("pe", tr, reads=[b_xst[k], b_const], writes=[b_ps[bank]])
                    evac_copy(xT[:, half * 4:half * 4 + 4, tt * 128:(tt + 1) * 128],
                              psf[bank][:, :].rearrange("p (q n) -> p q n", q=4), [b_ps[bank]], [b_xT])

            for l in range(depth):
                P.barrier()
                sq = [carve(0, 512), carve(512, 512)]
                b_sq = [B(), B()]
                rt = carve(1024, 512)
                b_rt = B()
                tmp = [carve(1536, 512), carve(2048, 512)]
                b_tmp = [B(), B()]
                for t in range(4):
                    ts = slice(t * 512, (t + 1) * 512)
                    stats_rstd(t, rt, b_rt, sq, b_sq)
                    for c in range(8):
                        k = c % 2
                        P.op("dve", TT(tmp[k], xT[:, c, ts], rt, ALU.mult), reads=[b_xT, b_rt], writes=[b_tmp[k]])
                        P.op("act", ACT(hT[:, c, ts], tmp[k], AF.Identity, scale=aT[:, l, c, b:b + 1], bias=modT[:, l, c, b:b + 1]),
                             reads=[b_tmp[k], b_small, b_mod], writes=[b_hT])

                P.barrier()
                sza = carve(0, S, BF16)
                Uacc = carve(1024, S)
                Lacc = carve(3072, S)
                pT = [carve(5120, 512, BF16), carve(5376, 512, BF16)]
                rl = carve(5632, 512)
                qkv = []
                for i in range(2):
                    qkv.append(dict(q=yret[:, 4 * i, :], k=yret[:, 4 * i + 1, :], vT=yret[:, 4 * i + 2, :],
                                    v=yret[:, 4 * i + 3, :].rearrange("p (k n) -> p k n", k=16),
                                    bq=B(), bk=B(), bvT=B(), bv=B()))
                b_pT = [B(), B()]
                b_sza, b_U, b_L, b_rl = B(), B(), B(), B()
                gi = 0
                sc_i = 0
                DILS = (1, 4, 16)

                def ZA(s):
                    slot = wload(win_d[l, :, 4608 + s * 128: 4608 + (s + 1) * 128], 8)
                    for t in range(4):
                        bank = t % 2
                        proj_feat(slot, t, bank)
                        P.op("act", ACT(sza[:, t * 512:(t + 1) * 512], psf[bank][:, :], AF.Silu),
                             reads=[b_ps[bank]], writes=[b_sza])

                def PROJ(s, g, Q):
                    dil = DILS[g]
                    for name, off, bb, scale in (("q", 0, Q["bq"], 128 ** -0.5), ("k", 1536, Q["bk"], None),
                                                 ("vT", 3072, Q["bvT"], None)):
                        c0 = off + g * 512 + s * 128
                        slot = wload(win_d[l, :, c0:c0 + 128], 8)
                        dst = Q[name]
                        for t in range(4):
                            bank = t % 2
                            proj_feat(slot, t, bank)
                            m0 = t * 512 // dil
                            mw = 512 // dil
                            if dil == 1:
                                o_ap = dst[:, t * 512:(t + 1) * 512]
                                i_ap = psf[bank][:, :]
                            else:
                                o_ap = dst.rearrange("p (r m) -> p r m", r=dil)[:, :, m0:m0 + mw]
                                i_ap = psf[bank][:, :].rearrange("p (m r) -> p r m", r=dil)
                            evac_copy(o_ap, i_ap, [b_ps[bank]], [bb], scale=scale)

                def CORE(s, g, Q):
                    dil = DILS[g]
                    L = S // dil
                    CL = L // 128
                    for q4 in range(4):
                        tr = [TR(psb[:, q * 128:(q + 1) * 128], Q["vT"][:, (q4 * 4 + q) * 128:(q4 * 4 + q + 1) * 128], identb)
                              for q in range(4)]
                        P.op("pe", tr, reads=[Q["bvT"], b_cb], writes=[b_psb])
                        evac_copy(Q["v"][:, q4 * 4:q4 * 4 + 4, :], psb[:, 0:512].rearrange("p (q n) -> p q n", q=4),
                                  [b_psb], [Q["bv"]])
                    steps = []
                    for Bk in range(4):
                        q_lo = 4 * Bk
                        kb_lo = q_lo - 1 if (q_lo % CL) != 0 else q_lo
                        for kb in range(kb_lo, q_lo + 4):
                            qbs = []
                            if kb >= q_lo:
                                qbs.append(kb)
                            if (kb + 1) % CL != 0 and kb + 1 <= q_lo + 3:
                                qbs.append(kb + 1)
                            steps.append(dict(Bk=Bk, kb=kb, n=128 * len(qbs), q0=qbs[0], m_lo=(0 if qbs[0] == kb else 128),
                                              first=(kb == kb_lo), last=(kb == q_lo + 3), col=(qbs[0] - q_lo) * 128))

                    def emit_sc(st, i, Q=Q):
                        sb_i = i % 2
                        bankS = 2 + sb_i
                        n, kb, q0, m_lo = st["n"], st["kb"], st["q0"], st["m_lo"]
                        P.op("pe", [MM(psf[bankS][:, 0:n], Q["k"][:, kb * 128:(kb + 1) * 128],
                                       Q["q"][:, q0 * 128:q0 * 128 + n], start=True, stop=False),
                                    MM(psf[bankS][:, 0:n], identb, maskb[:, m_lo:m_lo + n], start=False, stop=True)],
                             reads=[Q["bq"], Q["bk"], b_cb], writes=[b_ps[bankS]])
                        P.op("act", ACT(pT[sb_i][:, 0:n], psf[bankS][:, 0:n], AF.Exp),
                             reads=[b_ps[bankS]], writes=[b_pT[sb_i]])

                    def emit_pv(st, i, Q=Q, dil=dil, g=g):
                        sb_i = i % 2
                        n, kb, col, first, last, Bk = st["n"], st["kb"], st["col"], st["first"], st["last"], st["Bk"]
                        if Bk % 2 == 0:
                            pO, bO, pL, bL = psf[4], b_ps[4], psf[5], b_ps[5]
                        else:
                            pO, bO, pL, bL = psf[6], b_ps[6], psbf, b_psb
                        P.op("pe", [MM(pO[:, col:col + n], Q["v"][:, kb, :], pT[sb_i][:, 0:n], start=first, stop=last, sg=True),
                                    MM(pL[:, col:col + n], onesb, pT[sb_i][:, 0:n], start=first, stop=last, sg=True)],
                             reads=[Q["bv"], b_pT[sb_i], b_cb], writes=[bO, bL])
                        if not last:
                            return
                        for acc, bacc, pbank, bbank in ((Uacc, b_U, pO, bO), (Lacc, b_L, pL, bL)):
                            if dil == 1:
                                o_ap = acc[:, Bk * 512:(Bk + 1) * 512]
                                i_ap = pbank[:, :]
                            elif dil == 4:
                                o_ap = acc.rearrange("p (m r) -> p r m", r=4)[:, Bk, :]
                                i_ap = pbank[:, :]
                            else:
                                o_ap = acc.rearrange("p (m r) -> p r m", r=16)[:, 4 * Bk:4 * Bk + 4, :]
                                i_ap = pbank[:, :].rearrange("p (r m) -> p r m", r=4)
                            if g == 0:
                                P.op("dve", CP(o_ap, i_ap), reads=[bbank], writes=[bacc])
                            else:
                                P.op("dve", TT(o_ap, i_ap, o_ap, ALU.add), reads=[bbank, bacc], writes=[bacc])

                    for i, st in enumerate(steps):
                        emit_sc(st, i)
                        if i >= 1:
                            emit_pv(steps[i - 1], i - 1)
                    emit_pv(steps[-1], len(steps) - 1)

                def COMBINE(s):
                    for t in range(4):
                        ts = slice(t * 512, (t + 1) * 512)
                        P.op("dve", RCP(rl, Lacc[:, ts]), reads=[b_L], writes=[b_rl])
                        P.op("dve", TT(rl, Uacc[:, ts], rl, ALU.mult), reads=[b_U, b_rl], writes=[b_rl])
                        P.op("dve", TT(yatt[:, s, ts], rl, sza[:, ts], ALU.mult), reads=[b_rl, b_sza], writes=[b_yatt])

                units = [(s, g) for s in range(4) for g in range(3)]
                PROJ(0, 0, qkv[0])
                for u, (s, g) in enumerate(units):
                    if u + 1 < len(units):
                        PROJ(units[u + 1][0], units[u + 1][1], qkv[(u + 1) % 2])
                    if g == 0:
                        ZA(s)
                    CORE(s, g, qkv[u % 2])
                    if g == 2:
                        COMBINE(s)

                P.barrier()
                HS = 512
                NCH = HS // 128
                cosT = carve(0, S)
                sinT = carve(2048, S)
                qT = carve(4096, 2 * HS, BF16).rearrange("p (e n) -> p e n", e=2)
                kT = carve(4608, 2 * HS, BF16).rearrange("p (e n) -> p e n", e=2)
                kd = carve(5120, NCH * 256, BF16).rearrange("p (c n) -> p c n", c=NCH)
                vr = carve(5632, NCH * 256, BF16).rearrange("p (c n) -> p c n", c=NCH)
                szr = carve(6144, NCH * 256, BF16).rearrange("p (c n) -> p c n", c=NCH)
                t1 = [carve(6656, 512), carve(7168, 512)]
                stf = carve(7680, 512)
                stb = [carve(8192, 512, BF16).rearrange("p (e n) -> p e n", e=2),
                       carve(8448, 512, BF16).rearrange("p (e n) -> p e n", e=2)]
                attS4 = carve(8704, NCH * 128, BF16).rearrange("p (c n) -> p c n", c=NCH)
                yr2 = carve(8960, 256)
                yr = carve(9216, 256, BF16)
                st6 = [carve(9344 + 16 * i, 8) for i in range(NCH)]
                mv = [carve(9344 + 16 * i + 8, 4) for i in range(NCH)]
                rs = [carve(9344 + 16 * i + 12, 1) for i in range(NCH)]
                nmr = [carve(9344 + 16 * i + 14, 1) for i in range(NCH)]
                b_cs = B()
                b_qT, b_kT, b_kd, b_vr, b_szr = B(), B(), B(), B(), B()
                b_t1 = [B(), B()]
                b_stf = B()
                b_stb = [B(), B()]
                b_attS4 = B()
                b_yr2, b_yr = B(), B()
                b_st = [B() for _ in range(NCH)]
                posi = stf.bitcast(I32)
                ang, ang2 = t1[0], t1[1]
                for t in range(4):
                    ts = slice(t * 512, (t + 1) * 512)
                    P.dma("sp", posi, pos_d[b:b + 1, ts].broadcast_to([128, 512]), ch_posi, writes=[b_stf])
                    P.op("dve", CP(ang, posi), reads=[b_stf], writes=[b_t1[0]])
                    P.op("dve", TS(ang, ang, theta, None, ALU.mult), reads=[b_t1[0], b_const], writes=[b_t1[0]])
                    MAGIC = 12582912.0
                    for dst, use_shift in ((sinT, False), (cosT, True)):
                        srcang = ang
                        bsrc = b_t1[0]
                        if use_shift:
                            P.op("dve", TS(stf, ang, PI / 2, None, ALU.add), reads=[b_t1[0]], writes=[b_stf])
                            srcang = stf
                            bsrc = b_stf
                        P.op("dve", TS(ang2, srcang, 1.0 / (2 * PI), MAGIC, ALU.mult, ALU.add), reads=[bsrc], writes=[b_t1[1]])
                        P.op("dve", TS(ang2, ang2, -MAGIC, None, ALU.add), reads=[b_t1[1]], writes=[b_t1[1]])
                        P.op("dve", STT(ang2, ang2, -2 * PI, srcang, ALU.mult, ALU.add), reads=[b_t1[1], bsrc], writes=[b_t1[1]])
                        P.op("act", ACT(dst[:, ts], ang2, AF.Sin, scale=0.999999), reads=[b_t1[1]], writes=[b_cs])
                rot_i = 0
                for h in range(4):
                    ring_align2()
                    wcol = lambda base, e: win_d[l, :, base + h * 256 + e * 128: base + h * 256 + (e + 1) * 128]
                    s_q = [wload(wcol(5120, e), 8) for e in range(2)]
                    s_k = [wload(wcol(6144, e), 8) for e in range(2)]
                    s_v = [wload(wcol(7168, e), 8) for e in range(2)]
                    s_z = [wload(wcol(8192, e), 8) for e in range(2)]
                    assert s_v[1] == s_v[0] + 1 and s_z[1] == s_z[0] + 1
                    assert HS == 512
                    qproj_done = set()

                    def QPROJ(hq, s_q=s_q):
                        proj_feat(s_q[0], hq, 0)
                        proj_feat(s_q[1], hq, 1)
                        qproj_done.add(hq)

                    for hf in range(S // HS):
                        for slots, dstT, bdst in ((s_q, qT, b_qT), (s_k, kT, b_kT)):
                            for tl in range(HS // 512):
                                t = hf * (HS // 512) + tl
                                ts = slice(t * 512, (t + 1) * 512)
                                tls = slice(tl * 512, (tl + 1) * 512)
                                if slots is s_q:
                                    pb = 0
                                    if hf not in qproj_done:
                                        QPROJ(hf)
                                else:
                                    pb = 2
                                    proj_feat(slots[0], t, pb)
                                    proj_feat(slots[1], t, pb + 1)
                                pA, pB = psf[pb][:, :], psf[pb + 1][:, :]
                                rd = [b_ps[pb], b_ps[pb + 1], b_cs]
                                P.op("dve", TT(t1[0], pA, cosT[:, ts], ALU.mult), reads=rd, writes=[b_t1[0]])
                                P.op("dve", TT(t1[1], pB, sinT[:, ts], ALU.mult), reads=rd, writes=[b_t1[1]])
                                P.op("dve", TT(dstT[:, 0, tls], t1[0], t1[1], ALU.subtract), reads=b_t1, writes=[bdst])
                                P.op("dve", TT(t1[0], pB, cosT[:, ts], ALU.mult), reads=rd, writes=[b_t1[0]])
                                P.op("dve", TT(t1[1], pA, sinT[:, ts], ALU.mult), reads=rd, writes=[b_t1[1]])
                                P.op("dve", TT(dstT[:, 1, tls], t1[0], t1[1], ALU.add), reads=b_t1, writes=[bdst])
                        def TOKPROJ(slots, dst, bdst, fn, hf=hf):
                            for c2 in range(NCH // 2):
                                bank = 4 + (c2 % 2)
                                mm = []
                                for q in range(2):
                                    tok0 = hf * HS + (c2 * 2 + q) * 128
                                    for c in range(8):
                                        mm.append(MM(psf[bank][:, q * 256:(q + 1) * 256], hT[:, c, tok0:tok0 + 128],
                                                     wring[:, slots[0]:slots[0] + 2, c, :], start=(c == 0), stop=(c == 7), sg=True))
                                P.op("pe", mm, reads=[b_hT, b_ring[slots[0]], b_ring[slots[1]]], writes=[b_ps[bank]])
                                o_ap = dst[:, c2 * 2:c2 * 2 + 2, :]
                                i_ap = psf[bank][:, :].rearrange("p (q n) -> p q n", q=2)
                                if fn is None:
                                    evac_copy(o_ap, i_ap, [b_ps[bank]], [bdst])
                                else:
                                    P.op("act", ACT(o_ap, i_ap, AF.Silu), reads=[b_ps[bank]], writes=[bdst])
                        TOKPROJ(s_v, vr, b_vr, None)
                        gcs = [hf * NCH + cc for cc in range(NCH)]
                        att_mm = []
                        for cc in range(NCH):
                            cs = slice(cc * 128, (cc + 1) * 128)
                            att_mm.append(MM(psf[6][:, cc * 128:(cc + 1) * 128], kT[:, 0, cs], qT[:, 0, cs], start=True, stop=False, sg=True))
                            att_mm.append(MM(psf[6][:, cc * 128:(cc + 1) * 128], kT[:, 1, cs], qT[:, 1, cs], start=False, stop=True, sg=True))
                        P.op("pe", att_mm, reads=[b_kT, b_qT], writes=[b_ps[6]])
                        P.op("dve", TT(attS4, psf[6][:, :].rearrange("p (c n) -> p c n", c=NCH),
                                       amask[:, h, :].unsqueeze(1).broadcast_to([128, NCH, 128]), ALU.mult),
                             reads=[b_ps[6], b_const], writes=[b_attS4])

                        for c2 in range(NCH // 2):
                            tr = []
                            for q in range(2):
                                cc = c2 * 2 + q
                                for e2 in range(2):
                                    tr.append(TR(psb[:, (q * 2 + e2) * 128:(q * 2 + e2 + 1) * 128], kT[:, e2, cc * 128:(cc + 1) * 128], identb))
                            P.op("pe", tr, reads=[b_kT, b_cb], writes=[b_psb])
                            P.op("act", ACT(kd[:, c2 * 2:c2 * 2 + 2, :], psb[:, 0:512].rearrange("p (q n) -> p q n", q=2),
                                            AF.Copy, scale=kdec[:, h:h + 1]), reads=[b_psb, b_const], writes=[b_kd])
                        def emit_kv(cc):
                            gc = gcs[cc]
                            if gc >= 15:
                                return
                            bank = 4 + (cc % 2)
                            P.op("pe", [MM(psf[bank][:, 0:256], kd[:, cc, 0:128], vr[:, cc, :], sg=True),
                                        MM(psf[bank][:, 256:512], kd[:, cc, 128:256], vr[:, cc, :], sg=True)],
                                 reads=[b_kd, b_vr], writes=[b_ps[bank]])
                            if gc == 0:
                                P.op("dve", CP(stf, psf[bank][:, :]), reads=[b_ps[bank]], writes=[b_stf])
                            else:
                                P.op("dve", STT(stf, stf, CDEC[h], psf[bank][:, :], ALU.mult, ALU.add),
                                     reads=[b_ps[bank], b_stf], writes=[b_stf])
                            P.op("act", ACT(stb[gc % 2], stf.rearrange("p (e n) -> p e n", e=2), AF.Copy),
                                 reads=[b_stf], writes=[b_stb[gc % 2]])

                        def emit_G(cc):
                            gc = gcs[cc]
                            cs = slice(cc * 128, (cc + 1) * 128)
                            mm = [MM(psf[cc][:, 0:256], attS4[:, cc, :], vr[:, cc, :], start=True, stop=(gc == 0))]
                            rd = [b_attS4, b_vr]
                            if gc > 0:
                                sbi = (gc - 1) % 2
                                mm.append(MM(psf[cc][:, 0:256], qT[:, 0, cs], stb[sbi][:, 0, :], start=False, stop=False))
                                mm.append(MM(psf[cc][:, 0:256], qT[:, 1, cs], stb[sbi][:, 1, :], start=False, stop=True))
                                rd += [b_qT, b_stb[sbi]]
                            P.op("pe", mm, reads=rd, writes=[b_ps[cc]])
                            P.op("dve", ("bn_stats", dict(out=st6[cc][:, 0:BN_S], in_=psf[cc][:, 0:256])), reads=[b_ps[cc]], writes=[b_st[cc]])
                            P.op("dve", ("bn_aggr", dict(out=mv[cc][:, 0:BN_A], in_=st6[cc][:, 0:BN_S])), reads=[b_st[cc]], writes=[b_st[cc]])
                            if USE_POW:
                                P.op("dve", TT(rs[cc], mv[cc][:, 1:2], qdec2[:, h:h + 1], ALU.mult), reads=[b_st[cc], b_const], writes=[b_st[cc]])
                                P.op("dve", TS(rs[cc], rs[cc], EPS, -0.5, ALU.add, ALU.pow), reads=[b_st[cc]], writes=[b_st[cc]])
                            else:
                                P.op("act", ACT(rs[cc], mv[cc][:, 1:2], AF.Sqrt, scale=qdec2[:, h:h + 1], bias=epsc),
                                     reads=[b_st[cc], b_const], writes=[b_st[cc]])
                                P.op("dve", RCP(rs[cc], rs[cc]), reads=[b_st[cc]], writes=[b_st[cc]])
                            P.op("dve", TT(rs[cc], rs[cc], qdec[:, h:h + 1], ALU.mult), reads=[b_st[cc], b_const], writes=[b_st[cc]])
                            P.op("dve", STT(nmr[cc], mv[cc][:, 0:1], -1.0, rs[cc], ALU.mult, ALU.mult), reads=[b_st[cc]], writes=[b_st[cc]])

                        def emit_tail(cc):
                            gc = gcs[cc]
                            P.op("act", ACT(yr2, psf[cc][:, 0:256], AF.Identity, scale=rs[cc], bias=nmr[cc]),
                                 reads=[b_ps[cc], b_st[cc]], writes=[b_yr2])
                            P.op("dve", TT(yr, yr2, szr[:, cc, :], ALU.mult), reads=[b_yr2, b_szr], writes=[b_yr])
                            P.op("pe", [TR(psb[:, 512:640], yr[:, 0:128], identb), TR(psb[:, 640:768], yr[:, 128:256], identb)],
                                 reads=[b_yr, b_cb], writes=[b_psb])
                            tok0 = gc * 128
                            for q in range(2):
                                P.op("act", ACT(yret[:, 2 * h + q, tok0:tok0 + 128], psb[:, 512 + q * 128:640 + q * 128],
                                                AF.Copy, scale=gwT[:, l, 2 * h + q:2 * h + q + 1]),
                                     reads=[b_psb, b_p5], writes=[b_yret])

                        emit_kv(0)
                        emit_G(0)
                        emit_kv(1)
                        emit_G(1)
                        TOKPROJ(s_z, szr, b_szr, AF.Silu)
                        emit_tail(0)
                        emit_kv(2)
                        emit_G(2)
                        emit_tail(1)
                        emit_kv(3)
                        emit_G(3)
                        if hf + 1 < S // HS:
                            QPROJ(hf + 1)
                        emit_tail(2)
                        emit_tail(3)

                P.barrier()
                mg = carve(0, 8 * S, BF16).rearrange("p (c n) -> p c n", c=8)
                sg1 = carve(8192, 512, BF16)
                sg_ = [sg1, sg1]
                m1 = [carve(8448, 512), carve(8960, 512)]
                bsg1 = B()
                b_mg, b_sg, b_m1 = B(), [bsg1, bsg1], [B(), B()]
                for j in range(8):
                    s_ga = wload(win_d[l, :, 9216 + j * 128: 9216 + (j + 1) * 128], 8)
                    s_gr = wload(win_d[l, :, 10240 + j * 128: 10240 + (j + 1) * 128], 8)
                    s_pa = wload(wpa_d[l, :, j * 128:(j + 1) * 128], 4)
                    s_pr = wload(wpr_d[l, :, j * 128:(j + 1) * 128], 8)
                    for t in range(4):
                        ts = slice(t * 512, (t + 1) * 512)
                        proj_feat(s_ga, t, 0)
                        proj_feat(s_gr, t, 1)
                        proj_feat(s_pa, t, 2, kc=4, src=yatt, bs=b_yatt)
                        proj_feat(s_pr, t, 3, kc=8, src=yret, bs=b_yret)
                        for i in range(2):
                            P.op("act", ACT(sg_[i], psf[i][:, :], AF.Sigmoid), reads=[b_ps[i]], writes=[b_sg[i]])
                            P.op("dve", TT(m1[i], psf[2 + i][:, :], sg_[i], ALU.mult), reads=[b_ps[2 + i], b_sg[i]], writes=[b_m1[i]])
                        P.op("dve", TT(mg[:, j, ts], m1[0], m1[1], ALU.add), reads=b_m1, writes=[b_mg])
                for j2 in range(8):
                    s_o = wload(wout_d[l, :, j2 * 128:(j2 + 1) * 128], 8)
                    for t in range(4):
                        ts = slice(t * 512, (t + 1) * 512)
                        bank = 4 + (t % 2)
                        proj_feat(s_o, t, bank, kc=8, src=mg, bs=b_mg)
                        P.op("dve", STT(xT[:, j2, ts], psf[bank][:, :], modT[:, l, 16 + j2, b:b + 1], xT[:, j2, ts], ALU.mult, ALU.add),
                             reads=[b_ps[bank], b_xT, b_mod], writes=[b_xT])

            P.barrier()
            sq = [carve(0, 512), carve(512, 512)]
            b_sq = [B(), B()]
            rt = carve(1024, 512)
            b_rt = B()
            tmp1 = carve(1536, 512)
            b_tmp1 = B()
            yT = carve(2048, 4096).rearrange("p (c n) -> p c n", c=8)
            b_yT = B()
            ost = [carve(6144, 1024), carve(7168, 1024)]
            b_ost = [B(), B()]
            oi = 0
            outs = [(True, y_d)] + ([(False, xo_d)] if want_xo else [])
            for t in range(4):
                ts = slice(t * 512, (t + 1) * 512)
                stats_rstd(t, rt, b_rt, sq, b_sq)
                for normed, dst_d in outs:
                    if normed:
                        for c in range(8):
                            P.op("dve", TT(tmp1, xT[:, c, ts], rt, ALU.mult), reads=[b_xT, b_rt], writes=[b_tmp1])
                            P.op("act", ACT(yT[:, c, :], tmp1, AF.Copy, scale=fnT[:, c:c + 1]), reads=[b_tmp1, b_p3], writes=[b_yT])
                    for tb in range(4):
                        k = oi % 2
                        oi += 1
                        for half in range(2):
                            bank = half
                            tr = []
                            for q in range(4):
                                c = half * 4 + q
                                src = yT[:, c, tb * 128:(tb + 1) * 128] if normed else xT[:, c, t * 512 + tb * 128: t * 512 + (tb + 1) * 128]
                                tr.append(TR(psf[bank][:, q * 128:(q + 1) * 128], src, identf))
                            P.op("pe", tr, reads=[b_yT if normed else b_xT, b_const], writes=[b_ps[bank]])
                            evac_copy(ost[k][:, half * 512:(half + 1) * 512], psf[bank][:, :], [b_ps[bank]], [b_ost[k]])
                        tok0 = t * 512 + tb * 128
                        P.dma("sp", dst_d[b, tok0:tok0 + 128, :], ost[k], ch_out[k], reads=[b_ost[k]])
        P.final_wait("sp", ch_out)
        with nc.Block() as block:
            P.replay(block)
    return nc


_CACHE = {}


def _get_prog(depth, nseq, want_xo):
    key = (depth, nseq, want_xo)
    if key not in _CACHE:
        _CACHE[key] = build(depth, nseq, want_xo)
    return _CACHE[key]


MODE = "fused"


def kernel(x, c, positions, norm_w, w_ada, b_ada, w_in, ret_gn_w, w_proj_attn, w_proj_ret, w_out, final_norm_w):
    x = np.asarray(x, dtype=np.float32)
    Bt = x.shape[0]
    depth = int(np.asarray(norm_w).shape[0])
    cf, cb, _ = host_consts()
    f = lambda a: np.ascontiguousarray(np.asarray(a, dtype=np.float32))
    c = f(c)
    positions = np.ascontiguousarray(np.asarray(positions, dtype=np.int32))
    norm_w, w_ada, b_ada, w_in, ret_gn_w = f(norm_w), f(w_ada), f(b_ada), f(w_in), f(ret_gn_w)
    w_proj_attn, w_proj_ret, w_out, final_norm_w = f(w_proj_attn), f(w_proj_ret), f(w_out), f(final_norm_w)

    def launch(xin, rows, nseq, lsl, want_xo):
        d = lsl.stop - lsl.start
        nc = _get_prog(d, nseq, want_xo)
        in_maps = []
        for i in range(NCORES):
            in_maps.append({
                "x": np.ascontiguousarray(xin[i]), "c": np.ascontiguousarray(c[rows[i]]),
                "pos": np.ascontiguousarray(positions[rows[i]]),
                "norm_w": norm_w[lsl], "w_ada": w_ada[lsl], "b_ada": b_ada[lsl], "w_in": w_in[lsl], "gn_w": ret_gn_w[lsl],
                "w_pa": w_proj_attn[lsl], "w_pr": w_proj_ret[lsl], "w_out": w_out[lsl], "fnw": final_norm_w,
                "cf": cf, "cb": cb})
        res = run_bass_kernel_spmd(nc, in_maps, core_ids=list(range(NCORES)))
        y = np.stack([r["y"] for r in res.results], axis=0)
        xo = np.stack([r["xo"] for r in res.results], axis=0) if want_xo else None
        return y, xo

    out = np.empty_like(x)
    if MODE == "fused":
        nseq = Bt // NCORES
        rows = np.arange(Bt).reshape(NCORES, nseq)
        y, _ = launch(x[rows], rows, nseq, slice(0, depth), False)
        out[rows] = y
        return out
    ngrp = Bt // NCORES
    for g in range(ngrp):
        rows = (g * NCORES + np.arange(NCORES)).reshape(NCORES, 1)
        if MODE == "layers4":
            y, _ = launch(x[rows], rows, 1, slice(0, depth), False)
        else:
            cur = x[rows]
            for l in range(depth):
                y, cur = launch(cur, rows, 1, slice(l, l + 1), True)
        out[rows] = y
    return out
```
